# Optimizing a Trainium2 kernel written in Bass

```python
import math
import jax, jax.numpy as jnp
from jax import lax
import numpy as np

D_MODEL = 1024
BATCH = 8
SEQ = 4096
DEPTH = 4

CHUNK = 64
Q_BLOCK = 2 * CHUNK

ATT_HEAD_DIM = 64
ATT_WIDTH = D_MODEL // 2
ATT_HEADS = ATT_WIDTH // ATT_HEAD_DIM
POOL_WINDOWS = (2, 4, 8, 16)
POOL_GROUPS = 4
POOL_WIDTH = D_MODEL // 4
POOL_GROUP_DIM = POOL_WIDTH // POOL_GROUPS
SSM_WIDTH = D_MODEL // 4
SSM_GROUP_DIM = 16
SSM_GROUPS = SSM_WIDTH // SSM_GROUP_DIM
SSM_STATE = 64
DT_MIN = 1e-3
DT_MAX = 1e-1
N_BRANCH = 3
N_EXPERTS = 32
TOP_K = 4
D_FF = D_MODEL
SWIGLU_LIMIT = 7.0
SWIGLU_ALPHA = 1.702
MOE_BLOCK = 256
LN_EPS = 1e-5
DEEPNORM_ALPHA = (2.0 * DEPTH) ** 0.25
DEEPNORM_BETA = (8.0 * DEPTH) ** -0.25

IN_SPLIT_SIZES = (ATT_WIDTH, ATT_WIDTH, ATT_WIDTH, ATT_HEADS, POOL_WIDTH, SSM_WIDTH, N_BRANCH * D_MODEL)
N_IN = 3 * ATT_WIDTH + ATT_HEADS + POOL_WIDTH + SSM_WIDTH + N_BRANCH * D_MODEL

kernel_name = "hybrid_fox_pool_s5_moe_deepnorm"


def _split_points():
    pts, acc = [], 0
    for s in IN_SPLIT_SIZES[:-1]:
        acc += s
        pts.append(acc)
    return pts


def _layer_norm(x, g, b):
    xf = x.astype(jnp.float32)
    mu = xf.mean(-1, keepdims=True)
    var = jnp.square(xf - mu).mean(-1, keepdims=True)
    y = (xf - mu) * lax.rsqrt(var + LN_EPS) * g.astype(jnp.float32) + b.astype(jnp.float32)
    return y.astype(x.dtype)


def _forgetting_attention(q, k, v, f_logit, b_f):
    B_, S_, H, Dh = q.shape
    log_f = jax.nn.log_sigmoid(f_logit.astype(jnp.float32) + b_f.astype(jnp.float32))
    cum = jnp.cumsum(log_f, axis=1)
    cum_k = cum.transpose(0, 2, 1)
    nb = S_ // Q_BLOCK
    scale = Dh ** -0.5
    kf = k.astype(jnp.float32)
    qb = q.reshape(B_, nb, Q_BLOCK, H, Dh).transpose(1, 0, 2, 3, 4)
    cb = cum.reshape(B_, nb, Q_BLOCK, H).transpose(1, 0, 3, 2)
    key_pos = jnp.arange(S_)

    def block(args):
        i, q_i, c_i = args
        s = jnp.einsum('bqhd,bkhd->bhqk', q_i.astype(jnp.float32), kf) * scale
        s = s + (c_i[..., :, None] - cum_k[..., None, :])
        q_pos = i * Q_BLOCK + jnp.arange(Q_BLOCK)
        s = jnp.where(key_pos[None, :] <= q_pos[:, None], s, -jnp.inf)
        p = jax.nn.softmax(s, axis=-1)
        return jnp.einsum('bhqk,bkhd->bqhd', p.astype(v.dtype), v)

    out = lax.map(block, (jnp.arange(nb), qb, cb))
    return out.transpose(1, 0, 2, 3, 4).reshape(B_, S_, H * Dh)


def _multiscale_pool(u, w_pool, pool_scale):
    B_, S_, _ = u.shape
    ug = u.reshape(B_, S_, POOL_GROUPS, POOL_GROUP_DIM).astype(jnp.float32)
    cs = jnp.cumsum(ug, axis=1)
    cs0 = jnp.pad(cs, ((0, 0), (1, 0), (0, 0), (0, 0)))
    win = jnp.array(POOL_WINDOWS, jnp.int32)
    t = jnp.arange(S_, dtype=jnp.int32)[:, None]
    start = jnp.maximum(t + 1 - win[None, :], 0)
    g_idx = jnp.arange(POOL_GROUPS)[None, :]
    lower = cs0[:, start, g_idx]
    count = jnp.minimum(t + 1, win[None, :]).astype(jnp.float32)
    pooled = (cs - lower) / count[None, :, :, None]
    mixed = (pooled - ug).astype(u.dtype)
    y = jnp.einsum('bsgc,gcd->bsgd', mixed, w_pool).reshape(B_, S_, POOL_WIDTH)
    return y * pool_scale


def _s5(u, lam_re, lam_im, log_dt, b_re, b_im, c_re, c_im, d_skip, w_glu, b_glu):
    f32 = jnp.float32
    B_, S_, _ = u.shape
    uf = u.reshape(B_, S_, SSM_GROUPS, SSM_GROUP_DIM).astype(f32)
    dt = jnp.exp(log_dt.astype(f32))[:, None]
    lr = lam_re.astype(f32)
    li = lam_im.astype(f32)
    mag = jnp.exp(lr * dt)
    ar = mag * jnp.cos(li * dt)
    ai = mag * jnp.sin(li * dt)
    den = lr * lr + li * li
    nr = ar - 1.0
    zr = (nr * lr + ai * li) / den
    zi = (ai * lr - nr * li) / den
    br = b_re.astype(f32)
    bi = b_im.astype(f32)
    bbr = zr[..., None] * br - zi[..., None] * bi
    bbi = zr[..., None] * bi + zi[..., None] * br
    xr = jnp.einsum('bsgh,gph->bsgp', uf, bbr)
    xi = jnp.einsum('bsgh,gph->bsgp', uf, bbi)
    a_r = jnp.broadcast_to(ar[None, None], (1, S_) + ar.shape)
    a_i = jnp.broadcast_to(ai[None, None], (1, S_) + ai.shape)

    def combine(e1, e2):
        a1r, a1i, b1r, b1i = e1
        a2r, a2i, b2r, b2i = e2
        return (a2r * a1r - a2i * a1i,
                a2r * a1i + a2i * a1r,
                a2r * b1r - a2i * b1i + b2r,
                a2r * b1i + a2i * b1r + b2i)

    _, _, hr, hi = lax.associative_scan(combine, (a_r, a_i, xr, xi), axis=1)
    y = (jnp.einsum('bsgp,ghp->bsgh', hr, c_re.astype(f32))
         - jnp.einsum('bsgp,ghp->bsgh', hi, c_im.astype(f32))
         + d_skip.astype(f32) * uf)
    y = jax.nn.gelu(y.reshape(B_, S_, SSM_WIDTH).astype(u.dtype))
    return y * jax.nn.sigmoid(y @ w_glu + b_glu)


def _moe(h, w_router, b_router, w_up, b_up, w_down, b_down):
    f32 = jnp.float32
    B_, S_, D = h.shape
    xt = h.reshape(-1, D)
    N = xt.shape[0]
    logits = (xt @ w_router).astype(f32) + b_router.astype(f32)
    top_val, top_idx = lax.top_k(logits, TOP_K)
    gate = jax.nn.softmax(top_val, axis=-1)
    M = N * TOP_K
    flat_e = top_idx.reshape(-1)
    order = jnp.argsort(flat_e)
    sorted_e = flat_e[order]
    counts = jnp.bincount(flat_e, length=N_EXPERTS)
    padded = (counts + MOE_BLOCK - 1) // MOE_BLOCK * MOE_BLOCK
    start = jnp.cumsum(counts) - counts
    pend = jnp.cumsum(padded)
    pstart = pend - padded
    dest = pstart[sorted_e] + jnp.arange(M, dtype=counts.dtype) - start[sorted_e]
    P = M + N_EXPERTS * MOE_BLOCK
    nblk = P // MOE_BLOCK
    row_tok = jnp.zeros((P,), jnp.int32).at[dest].set((order // TOP_K).astype(jnp.int32))
    row_w = jnp.zeros((P,), f32).at[dest].set(gate.reshape(-1)[order])
    blk_e = jnp.minimum(jnp.searchsorted(pend, jnp.arange(nblk) * MOE_BLOCK, side='right'), N_EXPERTS - 1)
    xb = xt[row_tok].reshape(nblk, MOE_BLOCK, D)

    def expert_block(args):
        x_b, e = args
        hu = x_b @ w_up[e] + b_up[e]
        x_glu = jnp.minimum(hu[:, ::2], SWIGLU_LIMIT)
        x_lin = jnp.clip(hu[:, 1::2], -SWIGLU_LIMIT, SWIGLU_LIMIT)
        act = x_glu * jax.nn.sigmoid(SWIGLU_ALPHA * x_glu) * (x_lin + 1.0)
        return act @ w_down[e] + b_down[e]

    yb = lax.map(expert_block, (xb, blk_e)).reshape(P, D)
    y = jnp.zeros((N, D), h.dtype).at[row_tok].add(yb * row_w[:, None].astype(h.dtype))
    return y.reshape(B_, S_, D)


def setup_inputs(seed: int = 0) -> dict:
    key = jax.random.key(seed)
    ks = jax.random.split(key, 32)
    L, D = DEPTH, D_MODEL
    G, H, P = SSM_GROUPS, SSM_GROUP_DIM, SSM_STATE

    def nrm(k, shape, scale):
        return jax.random.normal(k, shape, jnp.float32) * scale

    x = nrm(ks[0], (BATCH, SEQ, D), 1.0)
    col_scale = jnp.concatenate([
        jnp.ones((2 * ATT_WIDTH,), jnp.float32),
        jnp.full((ATT_WIDTH,), DEEPNORM_BETA, jnp.float32),
        jnp.ones((N_IN - 3 * ATT_WIDTH,), jnp.float32)])
    w_in = nrm(ks[1], (L, D, N_IN), D ** -0.5) * col_scale
    b_forget = jnp.linspace(1.0, 6.0, ATT_HEADS, dtype=jnp.float32)[None, :] + nrm(ks[2], (L, ATT_HEADS), 0.1)
    w_pool = nrm(ks[3], (L, POOL_GROUPS, POOL_GROUP_DIM, POOL_GROUP_DIM), POOL_GROUP_DIM ** -0.5)
    pool_scale = 1.0 + nrm(ks[4], (L, POOL_WIDTH), 0.1)
    ssm_lambda_re = -0.5 + nrm(ks[5], (L, G, P), 0.01)
    ssm_lambda_im = jnp.broadcast_to(math.pi * jnp.arange(P, dtype=jnp.float32), (L, G, P))
    ssm_log_dt = jax.random.uniform(ks[6], (L, G), jnp.float32, math.log(DT_MIN), math.log(DT_MAX))
    ssm_b_re = nrm(ks[7], (L, G, P, H), (2.0 * H) ** -0.5)
    ssm_b_im = nrm(ks[8], (L, G, P, H), (2.0 * H) ** -0.5)
    ssm_c_re = nrm(ks[9], (L, G, H, P), P ** -0.5)
    ssm_c_im = nrm(ks[10], (L, G, H, P), P ** -0.5)
    ssm_d = nrm(ks[11], (L, G, H), 1.0)
    w_glu = nrm(ks[12], (L, SSM_WIDTH, SSM_WIDTH), SSM_WIDTH ** -0.5)
    b_glu = nrm(ks[13], (L, SSM_WIDTH), 0.01)
    w_branch_a = nrm(ks[14], (L, ATT_WIDTH, D), ATT_WIDTH ** -0.5 * DEEPNORM_BETA)
    w_branch_b = nrm(ks[15], (L, POOL_WIDTH, D), POOL_WIDTH ** -0.5 * DEEPNORM_BETA)
    w_branch_c = nrm(ks[16], (L, SSM_WIDTH, D), SSM_WIDTH ** -0.5 * DEEPNORM_BETA)
    w_out = nrm(ks[17], (L, D, D), D ** -0.5 * DEEPNORM_BETA)
    ln1_g = 1.0 + nrm(ks[18], (L, D), 0.02)
    ln1_b = nrm(ks[19], (L, D), 0.02)
    w_router = nrm(ks[20], (L, D, N_EXPERTS), D ** -0.5)
    b_router = nrm(ks[21], (L, N_EXPERTS), 0.01)
    w_up = nrm(ks[22], (L, N_EXPERTS, D, 2 * D_FF), D ** -0.5)
    b_up = nrm(ks[23], (L, N_EXPERTS, 2 * D_FF), 0.01)
    w_down = nrm(ks[24], (L, N_EXPERTS, D_FF, D), D_FF ** -0.5 * DEEPNORM_BETA)
    b_down = nrm(ks[25], (L, N_EXPERTS, D), 0.01)
    ln2_g = 1.0 + nrm(ks[26], (L, D), 0.02)
    ln2_b = nrm(ks[27], (L, D), 0.02)
    return {"x": x, "w_in": w_in, "b_forget": b_forget, "w_pool": w_pool, "pool_scale": pool_scale,
            "ssm_lambda_re": ssm_lambda_re, "ssm_lambda_im": ssm_lambda_im, "ssm_log_dt": ssm_log_dt,
            "ssm_b_re": ssm_b_re, "ssm_b_im": ssm_b_im, "ssm_c_re": ssm_c_re, "ssm_c_im": ssm_c_im,
            "ssm_d": ssm_d, "w_glu": w_glu, "b_glu": b_glu, "w_branch_a": w_branch_a,
            "w_branch_b": w_branch_b, "w_branch_c": w_branch_c, "w_out": w_out,
            "ln1_g": ln1_g, "ln1_b": ln1_b, "w_router": w_router, "b_router": b_router,
            "w_up": w_up, "b_up": b_up, "w_down": w_down, "b_down": b_down,
            "ln2_g": ln2_g, "ln2_b": ln2_b}


def reference(x, w_in, b_forget, w_pool, pool_scale, ssm_lambda_re, ssm_lambda_im, ssm_log_dt,
              ssm_b_re, ssm_b_im, ssm_c_re, ssm_c_im, ssm_d, w_glu, b_glu, w_branch_a,
              w_branch_b, w_branch_c, w_out, ln1_g, ln1_b, w_router, b_router,
              w_up, b_up, w_down, b_down, ln2_g, ln2_b):
    B_, S_, D = x.shape
    split_pts = _split_points()
    for l in range(DEPTH):
        h = x @ w_in[l]
        q, k, v, f_logit, u_pool, u_ssm, g = jnp.split(h, split_pts, axis=-1)
        shp = (B_, S_, ATT_HEADS, ATT_HEAD_DIM)
        y_a = _forgetting_attention(q.reshape(shp), k.reshape(shp), v.reshape(shp), f_logit, b_forget[l])
        y_b = _multiscale_pool(u_pool, w_pool[l], pool_scale[l])
        y_c = _s5(u_ssm, ssm_lambda_re[l], ssm_lambda_im[l], ssm_log_dt[l], ssm_b_re[l], ssm_b_im[l],
                  ssm_c_re[l], ssm_c_im[l], ssm_d[l], w_glu[l], b_glu[l])
        gates = jax.nn.sigmoid(g.reshape(B_, S_, N_BRANCH, D))
        merged = (gates[:, :, 0] * (y_a @ w_branch_a[l])
                  + gates[:, :, 1] * (y_b @ w_branch_b[l])
                  + gates[:, :, 2] * (y_c @ w_branch_c[l]))
        mix = merged @ w_out[l]
        x = _layer_norm(DEEPNORM_ALPHA * x + mix, ln1_g[l], ln1_b[l])
        ffn = _moe(x, w_router[l], b_router[l], w_up[l], b_up[l], w_down[l], b_down[l])
        x = _layer_norm(DEEPNORM_ALPHA * x + ffn, ln2_g[l], ln2_b[l])
    return x
```

```python
import math
from contextlib import ExitStack

import numpy as np
import concourse.bass as bass
import concourse.mybir as mybir
from concourse.bass_utils import run_bass_kernel_spmd

F32 = mybir.dt.float32
BF16 = mybir.dt.bfloat16
I32 = mybir.dt.int32
AF = mybir.ActivationFunctionType
ALU = mybir.AluOpType

D = 1024
T = 4096
NL = 4
NT = T // 128
NB = T // 512
N_IN = 5128
NE = 32
CAP = 768
CT = CAP // 128
DUMMY = NE * CAP
ALPHA = (2.0 * NL) ** 0.25
LN_EPS = 1e-5
NEG = -30000.0

ENGS = ("pe", "act", "dve", "pool", "sp")


class Op:
    __slots__ = ("id", "eng", "fn", "deps", "dma", "need_sig", "sig", "is_mm")

    def __init__(self, id, eng, fn, deps, dma, is_mm):
        self.id = id
        self.eng = eng
        self.fn = fn
        self.deps = deps
        self.dma = dma
        self.need_sig = dma
        self.sig = None
        self.is_mm = is_mm


class Prog:
    def __init__(self, nc):
        self.nc = nc
        self.ops = []
        self.last_write = {}
        self.readers = {}
        self.n_dma_sems = {"sp": 8, "pool": 12, "act": 12}
        self.n_eng_sems = 4
        self.open_dmas = []
        self.nbar = 0

    def op(self, eng, fn, reads=(), writes=(), dma=False, mm=False, extra=()):
        deps = set(extra)
        for r in reads:
            w = self.last_write.get(r)
            if w is not None:
                deps.add(w)
        for w_ in writes:
            w = self.last_write.get(w_)
            if w is not None:
                deps.add(w)
            rd = self.readers.get(w_)
            if rd is not None:
                deps.update(rd[0].values())
                deps.update(rd[1])
        oid = len(self.ops)
        o = Op(oid, eng, fn, sorted(deps), dma, mm)
        self.ops.append(o)
        for d in o.deps:
            self.ops[d].need_sig = True
        for r in reads:
            rd = self.readers.setdefault(r, ({}, []))
            if dma:
                rd[1].append(oid)
            else:
                rd[0][eng] = oid
        for w_ in writes:
            self.last_write[w_] = oid
            self.readers[w_] = ({}, [])
        if dma:
            self.open_dmas.append(oid)
        return oid

    def pe(self, fn, reads=(), writes=()):
        return self.op("pe", fn, reads, writes, mm=True)

    def act(self, fn, reads=(), writes=()):
        return self.op("act", fn, reads, writes)

    def dve(self, fn, reads=(), writes=()):
        return self.op("dve", fn, reads, writes)

    def pool(self, fn, reads=(), writes=()):
        return self.op("pool", fn, reads, writes)

    def dma(self, fn, reads=(), writes=(), q="sp"):
        return self.op(q, fn, reads, writes, dma=True)

    def barrier(self, scr):
        k = self.nbar
        self.nbar += 1
        dm = list(self.open_dmas)
        self.open_dmas = []
        tags = []
        for e in ("pe", "act", "dve", "pool", "sp"):
            tg = ("bar", k, e)
            tags.append(tg)
            if e == "pe":
                self.op("pe", lambda en: en.matmul(scr["ps"], lhsT=scr["idb"][:, 0:2], rhs=scr["idb"][:, 0:2], start=True, stop=True),
                        writes=[tg], extra=dm)
            elif e == "act":
                self.op("act", lambda en: en.copy(out=scr["a"], in_=scr["c"]), writes=[tg], extra=dm)
            elif e == "dve":
                self.op("dve", lambda en: en.tensor_copy(out=scr["v"], in_=scr["c"]), writes=[tg], extra=dm)
            elif e == "pool":
                self.op("pool", lambda en: en.tensor_copy(out=scr["g"], in_=scr["c"]), writes=[tg], extra=dm)
            else:
                self.op("sp", lambda en: en.dma_start(out=scr["d1"], in_=scr["d0"]), writes=[tg], dma=True, extra=dm)
        self.open_dmas = []
        for e in ("pe", "act", "dve", "pool"):
            tg2 = ("bar2", k, e)
            if e == "pe":
                self.op("pe", lambda en: en.matmul(scr["ps"], lhsT=scr["idb"][:, 0:2], rhs=scr["idb"][:, 0:2], start=True, stop=True),
                        reads=tags, writes=[tg2])
            elif e == "act":
                self.op("act", lambda en: en.copy(out=scr["a"], in_=scr["c"]), reads=tags, writes=[tg2])
            elif e == "dve":
                self.op("dve", lambda en: en.tensor_copy(out=scr["v"], in_=scr["c"]), reads=tags, writes=[tg2])
            else:
                self.op("pool", lambda en: en.tensor_copy(out=scr["g"], in_=scr["c"]), reads=tags, writes=[tg2])
        self.op("sp", lambda en: en.dma_start(out=scr["d1"], in_=scr["d0"]), reads=tags, writes=[("bar2", k, "sp")], dma=True)

    def emit(self, final_wait_ops=()):
        nc = self.nc
        with ExitStack() as es:
            eng_sems = {e: [es.enter_context(nc.semaphore(f"s_{e}{k}")) for k in range(self.n_eng_sems)]
                        for e in ("pe", "act", "dve", "pool")}
            dma_sems = {q: [es.enter_context(nc.semaphore(f"d_{q}{k}")) for k in range(n)]
                        for q, n in self.n_dma_sems.items()}
            eng_cnt = {e: [0] * self.n_eng_sems for e in eng_sems}
            eng_rr = {e: 0 for e in eng_sems}
            dma_cnt = {q: [0] * n for q, n in self.n_dma_sems.items()}
            dma_rr = {q: 0 for q in dma_sems}
            dma_prev = {}
            for o in self.ops:
                if o.dma:
                    q = o.eng
                    k = dma_rr[q]
                    dma_rr[q] = (k + 1) % len(dma_sems[q])
                    prev = dma_cnt[q][k]
                    dma_cnt[q][k] += 16
                    o.sig = (dma_sems[q][k], dma_cnt[q][k], 16)
                    dma_prev[o.id] = (dma_sems[q][k], prev)
                elif o.need_sig:
                    e = o.eng
                    k = eng_rr[e]
                    eng_rr[e] = (k + 1) % self.n_eng_sems
                    eng_cnt[e][k] += 1
                    o.sig = (eng_sems[e][k], eng_cnt[e][k], 1)
            per_eng = {e: [o for o in self.ops if o.eng == e] for e in ENGS}
            ops = self.ops
            final_wait_ops = list(final_wait_ops)

            def run_engine(ename, eobj):
                waited = {}

                def wait(sem, val):
                    key = id(sem)
                    if waited.get(key, 0) >= val:
                        return
                    eobj.wait_ge(sem, val)
                    waited[key] = val

                for o in per_eng[ename]:
                    need = {}
                    for d in o.deps:
                        do = ops[d]
                        if do.eng == ename and do.is_mm and o.is_mm:
                            continue
                        sem, val, _ = do.sig
                        k_ = id(sem)
                        if k_ not in need or need[k_][1] < val:
                            need[k_] = (sem, val)
                    for sem, val in need.values():
                        wait(sem, val)
                    if o.dma:
                        sem, prev = dma_prev[o.id]
                        if prev > 0:
                            wait(sem, prev)
                    ins = o.fn(eobj)
                    if o.sig is not None:
                        sem, val, inc = o.sig
                        ins.then_inc(sem, inc)
                if ename == "sp":
                    for oid in final_wait_ops:
                        sem, val, _ = ops[oid].sig
                        wait(sem, val)

            with nc.Block() as block:
                @block.sync
                def _(e):
                    run_engine("sp", e)

                @block.tensor
                def _(e):
                    run_engine("pe", e)

                @block.scalar
                def _(e):
                    run_engine("act", e)

                @block.vector
                def _(e):
                    run_engine("dve", e)

                @block.gpsimd
                def _(e):
                    run_engine("pool", e)


class Arena:
    def __init__(self, ap, nwords):
        self.ap = ap
        self.n = nwords
        self.off = 0

    def reset(self, off=0):
        self.off = off

    def alloc(self, shape, dt, parts=None):
        esz = 4 if dt in (F32, I32) else 2
        free = int(np.prod(shape[1:]))
        words = (free * esz + 3) // 4
        words = (words + 7) // 8 * 8
        assert self.off + words <= self.n, f"SBUF arena overflow: need {self.off + words} > {self.n}"
        v = self.ap[0:shape[0], self.off:self.off + words]
        self.off += words
        if dt != F32:
            v = v.bitcast(dt)
        v = v[:, 0:free]
        if len(shape) == 3:
            v = v.rearrange("p (a b) -> p a b", a=shape[1])
        elif len(shape) == 4:
            v = v.rearrange("p (a b c) -> p a b c", a=shape[1], b=shape[2])
        return v


def build_nc(n_layers=NL, dbg=False, stop_after=None):
    nc = bass.Bass("TRN2", target_bir_lowering=False)
    L = NL

    def din(name, shape):
        return nc.dram_tensor(name, list(shape), F32, kind="ExternalInput").ap()

    x_in = din("x", [T, D])
    w_in = din("w_in", [L, D, N_IN])
    b_forget = din("b_forget", [L, 8])
    w_pool = din("w_pool", [L, 4, 64, 64])
    pool_scale = din("pool_scale", [L, 256])
    lam_re = din("ssm_lambda_re", [L, 16, 64])
    lam_im = din("ssm_lambda_im", [L, 16, 64])
    log_dt = din("ssm_log_dt", [L, 16])
    b_re = din("ssm_b_re", [L, 16, 64, 16])
    b_im = din("ssm_b_im", [L, 16, 64, 16])
    c_re = din("ssm_c_re", [L, 16, 16, 64])
    c_im = din("ssm_c_im", [L, 16, 16, 64])
    ssm_d = din("ssm_d", [L, 16, 16])
    w_glu = din("w_glu", [L, 256, 256])
    b_glu = din("b_glu", [L, 256])
    w_br = [din("w_branch_a", [L, 512, D]), din("w_branch_b", [L, 256, D]), din("w_branch_c", [L, 256, D])]
    w_out = din("w_out", [L, D, D])
    ln_g = [din("ln1_g", [L, D]), din("ln2_g", [L, D])]
    ln_b = [din("ln1_b", [L, D]), din("ln2_b", [L, D])]
    w_router = din("w_router", [L, D, NE])
    b_router = din("b_router", [L, NE])
    w_up = din("w_up", [L, NE, D, 2 * D])
    b_up = din("b_up", [L, NE, 2 * D])
    w_down = din("w_down", [L, NE, D, D])
    b_down = din("b_down", [L, NE, D])
    out = nc.dram_tensor("out", [T, D], F32, kind="ExternalOutput").ap()

    skind = "ExternalOutput" if dbg else "Internal"

    def dscr(name, shape, dt):
        return nc.dram_tensor(name, list(shape), dt, kind=skind).ap()

    xs = dscr("xs", [T, D], F32)
    xT_d = dscr("xT_d", [128, 8, T], BF16)
    qT_d = dscr("qT_d", [512, T], BF16)
    kT_d = dscr("kT_d", [512, T], BF16)
    v_d = dscr("v_d", [T, 512], BF16)
    c_d = dscr("c_d", [8, T], F32)
    ya_d = dscr("ya_d", [128, 4, T], BF16)
    yb_d = dscr("yb_d", [128, 2, T], BF16)
    yc_d = dscr("yc_d", [128, 2, T], BF16)
    x1_d = dscr("x1_d", [T, D], F32)
    xg_d = dscr("xg_d", [NE * CAP + 128, D], BF16)
    yg_d = dscr("yg_d", [NE * CAP + 128, D], F32)
    bar_d = nc.dram_tensor("bar_d", [2, 16], F32, kind="Internal").ap()

    P = Prog(nc)
    es = ExitStack()
    with es:
        NW = 52900
        arena_t = es.enter_context(nc.sbuf_tensor("arena", [128, NW], F32))
        misc = es.enter_context(nc.sbuf_tensor("misc", [128, 64], F32))
        PS = [es.enter_context(nc.psum_tensor(f"ps{k}", [128, 512], F32)) for k in range(8)]
        A = Arena(arena_t, NW)

        def psb(k):
            return PS[k][:, :].bitcast(BF16).rearrange("p (a b) -> p a b", a=8)

        identf = A.alloc([128, 128], F32)
        identb = A.alloc([128, 128], BF16)
        onesb = A.alloc([128, 128], BF16)
        lstrict = A.alloc([128, 128], BF16)
        onesf = A.alloc([128, 512], F32)
        eC = A.alloc([128, NE], F32)
        invc = A.alloc([128, 2, 16], F32)
        iota512 = A.alloc([128, 512], F32)
        masks = A.alloc([128, NT, NE], BF16)
        idxs = A.alloc([128, NT, 4], I32)
        wsels = A.alloc([128, NT, 4], F32)
        WdTs = A.alloc([32, NT, 128], F32)
        eps_c = A.alloc([128, 1], F32)
        PERSIST_C = A.off
        fT = A.alloc([8, T], F32)
        upT = A.alloc([128, 2, 16 + T], F32)
        usT = A.alloc([128, 2, T], BF16)
        PERSIST = A.off

        scr = {"ps": PS[7][0:2, 0:2], "idb": identb, "a": misc[0:1, 0:1], "v": misc[0:1, 1:2], "g": misc[0:1, 2:3],
               "c": misc[0:1, 8:9], "d0": bar_d[0:1, :], "d1": bar_d[1:2, :]}

        P.pool(lambda e: e.memset(misc[:, :], 0.0), writes=["misc"])
        P.pool(lambda e: e.memset(onesf, 1.0), writes=["onesf"])
        P.pool(lambda e: e.memset(eps_c, LN_EPS), writes=["eps_c"])
        P.pool(lambda e: e.affine_select(out=identf, in_=onesf[:, 0:128], pattern=[[-1, 128]], compare_op=ALU.is_equal,
                                         fill=0.0, base=0, channel_multiplier=1), reads=["onesf"], writes=["identf"])
        P.dve(lambda e: e.tensor_copy(out=identb, in_=identf), reads=["identf"], writes=["identb"])
        P.dve(lambda e: e.tensor_copy(out=onesb, in_=onesf[:, 0:128]), reads=["onesf"], writes=["onesb"])
        P.pool(lambda e: e.affine_select(out=lstrict, in_=onesb, pattern=[[1, 128]], compare_op=ALU.is_gt,
                                         fill=0.0, base=0, channel_multiplier=-1), reads=["onesb"], writes=["lstrict"])
        P.pool(lambda e: e.iota(eC, pattern=[[CAP, NE]], base=0, channel_multiplier=0, allow_small_or_imprecise_dtypes=True),
               writes=["eC"])
        P.pool(lambda e: e.iota(iota512, pattern=[[1, 512]], base=1, channel_multiplier=0, allow_small_or_imprecise_dtypes=True),
               writes=["iota512"])
        for pc in range(2):
            for hf in range(2):
                w = 2 ** (2 * pc + hf + 1)
                sl = slice(hf * 64, hf * 64 + 64)
                P.dve(lambda e, pc=pc, sl=sl, w=w: e.tensor_scalar(out=invc[sl, pc, :], in0=iota512[sl, 0:16], scalar1=float(w),
                                                                  scalar2=None, op0=ALU.min),
                      reads=["iota512"], writes=[("invc", pc, hf)])
                P.dve(lambda e, pc=pc, sl=sl: e.reciprocal(out=invc[sl, pc, :], in_=invc[sl, pc, :]),
                      reads=[("invc", pc, hf)], writes=[("invc", pc, hf)])
        P.pool(lambda e: e.memset(upT[:, :, 0:16], 0.0), writes=["upT_pad"])
        P.barrier(scr)

        def dma(out, in_, r=(), w=(), q="sp"):
            return P.dma(lambda e: e.dma_start(out=out, in_=in_), r, w, q)

        def dma_slow(out, in_, r=(), w=(), q="sp"):
            return P.dma(lambda e: e.dma_start(out=out, in_=in_, allow_slow_non_contiguous=True), r, w, q)

        def mm(out, lhsT, rhs, st, sp, r, w):
            return P.pe(lambda e: e.matmul(out, lhsT=lhsT, rhs=rhs, start=st, stop=sp), r, w)

        def tr(out, in_, ident, r, w):
            return P.pe(lambda e: e.transpose(out=out, in_=in_, identity=ident), r, w)

        def cp(eng, out, in_, r, w):
            if eng == "act":
                return P.act(lambda e: e.copy(out=out, in_=in_), r, w)
            return P.op(eng, lambda e: e.tensor_copy(out=out, in_=in_), r, w)

        def actf(out, in_, func, r, w, bias=None, scale=None):
            kw = {}
            if bias is not None:
                kw["bias"] = bias
            if scale is not None:
                kw["scale"] = scale
            return P.act(lambda e: e.activation(out=out, in_=in_, func=func, **kw), r, w)

        def ts(eng, out, in0, s1, s2, op0, op1, r, w):
            if op1 is None:
                return P.op(eng, lambda e: e.tensor_scalar(out=out, in0=in0, scalar1=s1, scalar2=None, op0=op0), r, w)
            return P.op(eng, lambda e: e.tensor_scalar(out=out, in0=in0, scalar1=s1, scalar2=s2, op0=op0, op1=op1), r, w)

        def tt(eng, out, in0, in1, op, r, w):
            return P.op(eng, lambda e: e.tensor_tensor(out=out, in0=in0, in1=in1, op=op), r, w)

        def stt(eng, out, in0, scalar, in1, op0, op1, r, w, accum=None):
            if accum is None:
                return P.op(eng, lambda e: e.scalar_tensor_tensor(out=out, in0=in0, scalar=scalar, in1=in1, op0=op0, op1=op1), r, w)
            return P.op(eng, lambda e: e.scalar_tensor_tensor(out=out, in0=in0, scalar=scalar, in1=in1, op0=op0, op1=op1,
                                                              accum_out=accum), r, w)

        def memset(eng, out, val, w):
            return P.op(eng, lambda e: e.memset(out, val), (), w)

        fin_ops = []
        rr = [0]
        regc = {}

        def negreg(e):
            if "neg" not in regc:
                regc["neg"] = e.to_reg(NEG)
            return regc["neg"]

        def nxt(n):
            rr[0] += 1
            return rr[0] % n

        for l in range(n_layers):
            xsrc = x_in if l == 0 else xs
            xdst = out if l == n_layers - 1 else xs

            A.reset(PERSIST)
            NA = 2056
            wA = A.alloc([128, 8, NA], BF16)
            xb = [A.alloc([128, D], BF16) for _ in range(3)]
            xTb = [A.alloc([128, 8, 512], BF16) for _ in range(2)]
            qst = [A.alloc([128, 4, 512], BF16) for _ in range(2)]
            vst = [A.alloc([128, 512], BF16) for _ in range(2)]
            memset("pool", upT[:, :, 0:16], 0.0, ["upT_pad"])
            groups = [(0, 512), (512, 512), (1024, 512), (1536, 520)]
            for gi, (c0, n) in enumerate(groups):
                dma(wA[:, :, c0:c0 + n], w_in[l][:, c0:c0 + n].rearrange("(kc p) n -> p kc n", p=128), w=[("wA", gi, 0), ("wA", gi, 1)], q="pool")
            wA_tags = [("wA", gi, h) for gi in range(4) for h in range(2)]
            psr = 0
            for tb in range(NB):
                s2 = tb % 2
                tbs = slice(tb * 512, (tb + 1) * 512)
                for ti in range(4):
                    i = tb * 4 + ti
                    s = i % 3
                    dma(xb[s], xsrc[i * 128:(i + 1) * 128, :], w=[("xb", s)], q="pool")
                    pk = 6 + (i % 2)
                    for kc in range(8):
                        tr(psb(pk)[:, kc, :], xb[s][:, kc * 128:(kc + 1) * 128], identb, [("xb", s), "identb"], [("ps", pk)])
                    cp("act" if i % 2 == 0 else "dve", xTb[s2][:, :, ti * 128:(ti + 1) * 128], psb(pk), [("ps", pk)], [("xTb", s2)])
                dma(xT_d[:, :, tbs], xTb[s2], [("xTb", s2)], ["xT_d"])
                rdA = wA_tags + [("xTb", s2)]
                for qi, (dst, scale) in enumerate(((qT_d, 0.125), (kT_d, 1.0))):
                    for sc in range(4):
                        pk = psr % 6
                        psr += 1
                        c = qi * 512 + sc * 128
                        for kc in range(8):
                            mm(PS[pk][:, :], wA[:, kc, c:c + 128], xTb[s2][:, kc, :], kc == 0, kc == 7, rdA, [("ps", pk)])
                        if sc % 2 == 0:
                            actf(qst[qi][:, sc, :], PS[pk][:, :], AF.Copy, [("ps", pk)], [("qst", qi)], scale=scale)
                        else:
                            ts("dve", qst[qi][:, sc, :], PS[pk][:, :], scale, None, ALU.mult, None, [("ps", pk)], [("qst", qi)])
                    dma(dst.rearrange("(sc p) t -> p sc t", p=128)[:, :, tbs], qst[qi], [("qst", qi)], [("qk_d", qi)])
                for ti in range(4):
                    pk = psr % 6
                    psr += 1
                    for kc in range(8):
                        mm(PS[pk][:, :], xTb[s2][:, kc, ti * 128:(ti + 1) * 128], wA[:, kc, 1024:1536], kc == 0, kc == 7, rdA, [("ps", pk)])
                    cp("act" if ti % 2 == 0 else "dve", vst[ti % 2], PS[pk][:, :], [("ps", pk)], [("vst", ti % 2)])
                    dma(v_d[(tb * 4 + ti) * 128:(tb * 4 + ti + 1) * 128, :], vst[ti % 2], [("vst", ti % 2)], ["v_d"])
                pk = psr % 6
                psr += 1
                for kc in range(8):
                    mm(PS[pk][0:8, :], wA[:, kc, 1536:1544], xTb[s2][:, kc, :], kc == 0, kc == 7, rdA, [("ps", pk)])
                cp("act", fT[:, tbs], PS[pk][0:8, :], [("ps", pk)], [("fT", tb)])
                for pc in range(2):
                    pk = psr % 6
                    psr += 1
                    c = 1544 + pc * 128
                    for kc in range(8):
                        mm(PS[pk][:, :], wA[:, kc, c:c + 128], xTb[s2][:, kc, :], kc == 0, kc == 7, rdA, [("ps", pk)])
                    cp("act", upT[:, pc, 16 + tb * 512:16 + (tb + 1) * 512], PS[pk][:, :], [("ps", pk)], [("upT", pc)])
                for sc in range(2):
                    pk = psr % 6
                    psr += 1
                    c = 1800 + sc * 128
                    for kc in range(8):
                        mm(PS[pk][:, :], wA[:, kc, c:c + 128], xTb[s2][:, kc, :], kc == 0, kc == 7, rdA, [("ps", pk)])
                    cp("dve", usT[:, sc, tbs], PS[pk][:, :], [("ps", pk)], [("usT", sc)])
            P.barrier(scr)
            if stop_after == "A":
                break

            A.reset(PERSIST)
            bfc = A.alloc([8, 1], F32)
            nbf = A.alloc([8, 1], F32)
            ef = fT
            cs = A.alloc([8, T], F32)
            ncs = fT
            csK = A.alloc([128, NT, 8], F32)
            dma(bfc, b_forget[l].rearrange("(h o) -> h o", o=1), w=["bfc"])
            ts("dve", nbf, bfc, -1.0, None, ALU.mult, None, ["bfc"], ["nbf"])
            fr = [("fT", tb) for tb in range(NB)]
            actf(ef, fT, AF.Exp, fr + ["nbf"], fr + ["ef"], bias=nbf, scale=-1.0)
            actf(ef, ef, AF.Ln, ["ef"], ["ef"], bias=1.0, scale=1.0)
            for hh in range(2):
                hs = slice(hh * 2048, (hh + 1) * 2048)
                init = 0.0 if hh == 0 else cs[:, 2047:2048]
                P.dve(lambda e, hs=hs, init=init: e.tensor_tensor_scan(out=cs[:, hs], data0=onesf[0:8, 0:1].to_broadcast([8, 2048]),
                                                                         data1=ef[:, hs], initial=init, op0=ALU.mult, op1=ALU.add),
                      ["ef", "onesf", "cs"], ["cs"])
            ts("dve", ncs, cs, -1.0, None, ALU.mult, None, ["cs", "ef"], ["ncs", "ef"] + fr)
            dma(c_d, ncs, ["ncs"], ["c_d"])
            for i in range(NT):
                tr(PS[5][:, i * 8:(i + 1) * 8], cs[:, i * 128:(i + 1) * 128], identf[0:8, 0:8], ["cs", "identf"], [("ps", 5)])
            cp("dve", csK, PS[5][:, 0:NT * 8].rearrange("p (i h) -> p i h", h=8), [("ps", 5)], ["csK"])

            qh = [A.alloc([64, T], BF16) for _ in range(2)]
            kh = [A.alloc([64, T], BF16) for _ in range(2)]
            V1 = [A.alloc([128, NT, 128], BF16) for _ in range(2)]
            cB0 = A.alloc([128, T], F32)
            cB = [cB0, cB0]
            NBUF = 4
            tmpb = [A.alloc([128, 512], F32) for _ in range(NBUF)]
            Ptb = [A.alloc([128, 512], BF16) for _ in range(NBUF)]
            rden = [A.alloc([64, 512], F32) for _ in range(2)]
            yst = [A.alloc([64, 512], BF16) for _ in range(2)]
            for s in range(2):
                memset("pool", V1[s][:, :, 64:128], 1.0, [("V1o", s)])
            steps = [(h, qb, j) for h in range(8) for qb in range(NB) for j in range(4 * qb + 4)]
            LA = 2

            def emit_front(t):
                h, qb, j = steps[t]
                s = h % 2
                if qb == 0 and j == 0:
                    dma(qh[s], qT_d[h * 64:(h + 1) * 64, :], [("qk_d", 0)], [("qh", s)])
                    dma(kh[s], kT_d[h * 64:(h + 1) * 64, :], [("qk_d", 1)], [("kh", s)])
                    dma(V1[s][:, :, 0:64], v_d.rearrange("(i p) c -> p i c", p=128)[:, :, h * 64:(h + 1) * 64], ["v_d"], [("V1", s)])
                    dma(cB[s], c_d[h:h + 1, :].partition_broadcast(128), ["c_d"], [("cB", 0)])
                qs = slice(qb * 512, (qb + 1) * 512)
                pk = t % 4
                sb = t % NBUF
                mm(PS[pk][:, :], kh[s][:, j * 128:(j + 1) * 128], qh[s][:, qs], True, True, [("qh", s), ("kh", s)], [("ps", pk)])
                tt("dve", tmpb[sb], PS[pk][:, :], cB[s][:, qs], ALU.add, [("ps", pk), ("cB", 0)], [("tmp", sb)])
                if j >= 4 * qb:
                    P.pool(lambda e, sb=sb, base=qb * 512 - j * 128: e.affine_select(
                        out=tmpb[sb], in_=tmpb[sb], pattern=[[1, 512]], compare_op=ALU.is_ge, fill=negreg(e),
                        base=base, channel_multiplier=-1), [("tmp", sb)], [("tmp", sb)])
                actf(Ptb[sb], tmpb[sb], AF.Exp, [("tmp", sb), "csK"], [("Pt", sb)], bias=csK[:, j, h:h + 1], scale=1.0)

            def emit_back(t):
                h, qb, j = steps[t]
                s = h % 2
                qs = slice(qb * 512, (qb + 1) * 512)
                sb = t % NBUF
                po = 4 + (qb % 2)
                nj = 4 * qb + 4
                mm(PS[po][:, :], V1[s][:, j, :], Ptb[sb], j == 0, j == nj - 1, [("V1", s), ("V1o", s), ("Pt", sb)], [("ps", po)])
                if j == nj - 1:
                    r2 = qb % 2
                    P.dve(lambda e, r2=r2, po=po: e.reciprocal(out=rden[r2], in_=PS[po][64:128, :]), [("ps", po)], [("rden", r2)])
                    tt("dve", yst[r2], PS[po][0:64, :], rden[r2], ALU.mult, [("ps", po), ("rden", r2)], [("yst", r2)])
                    dma(ya_d[(h % 2) * 64:(h % 2) * 64 + 64, h // 2, qs], yst[r2], [("yst", r2)], ["ya_d"])

            for t in range(len(steps) + LA):
                if t < len(steps):
                    emit_front(t)
                if t >= LA:
                    emit_back(t - LA)
            P.barrier(scr)
            if stop_after == "B2":
                break
            A.reset(PERSIST)
            wpf = A.alloc([128, 2, 128], F32)
            wpb = A.alloc([128, 2, 128], BF16)
            psc = A.alloc([128, 2], F32)
            sbuf4 = [A.alloc([128, 16 + T], F32) for _ in range(4)]
            mixT = A.alloc([128, 2, T], BF16)
            tmp16 = A.alloc([128, 2, 16], F32)
            ybst = [A.alloc([128, 512], BF16) for _ in range(2)]
            memset("pool", wpf, 0.0, ["wpf"])
            for g in range(4):
                hs = slice((g % 2) * 64, (g % 2) * 64 + 64)
                dma(wpf[hs, g // 2, (g % 2) * 64:(g % 2) * 64 + 64], w_pool[l, g], w=["wpf"])
            cp("dve", wpb, wpf, ["wpf"], ["wpb"])
            dma_slow(psc, pool_scale[l].rearrange("(c p) -> p c", p=128), w=["psc"])
            for pc in range(2):
                eng = "dve" if pc == 0 else "pool"
                a_, b_ = sbuf4[2 * pc], sbuf4[2 * pc + 1]
                ta, tb_ = ("sA", pc), ("sB", pc)
                memset(eng, a_[:, 0:16], 0.0, [ta])
                memset(eng, b_[:, 0:16], 0.0, [tb_])
                u = upT[:, pc, :]
                ut = ("upT", pc)
                tt(eng, a_[:, 16:], u[:, 16:], u[:, 15:15 + T], ALU.add, [ut, "upT_pad"], [ta])
                tt(eng, b_[:, 16:], a_[:, 16:], a_[:, 14:14 + T], ALU.add, [ta], [tb_])
                if pc == 1:
                    tt(eng, a_[:, 16:], b_[:, 16:], b_[:, 12:12 + T], ALU.add, [tb_], [ta])
                    tt(eng, b_[:, 16:], a_[:, 16:], a_[:, 8:8 + T], ALU.add, [ta], [tb_])
                for hf in range(2):
                    sl = slice(hf * 64, hf * 64 + 64)
                    src, st_ = (a_, ta) if hf == 0 else (b_, tb_)
                    w = 2 ** (2 * pc + hf + 1)
                    stt("dve", mixT[sl, pc, :], src[sl, 16:], 1.0 / w, u[sl, 16:], ALU.mult, ALU.subtract, [st_, ut], [("mixT", pc, hf)])
                    tt(eng, tmp16[sl, pc, :], src[sl, 16:32], invc[sl, pc, :], ALU.mult, [st_, ("invc", pc, hf)], [("tmp16", pc, hf)])
                    tt(eng, mixT[sl, pc, 0:16], tmp16[sl, pc, :], u[sl, 16:32], ALU.subtract, [("tmp16", pc, hf), ut], [("mixT", pc, hf)])
            for pc in range(2):
                for tb in range(NB):
                    tbs = slice(tb * 512, (tb + 1) * 512)
                    pk = nxt(4)
                    s = nxt(2)
                    mm(PS[pk][:, :], wpb[:, pc, :], mixT[:, pc, tbs], True, True, ["wpb", ("mixT", pc, 0), ("mixT", pc, 1)], [("ps", pk)])
                    actf(ybst[s], PS[pk][:, :], AF.Copy, [("ps", pk), "psc"], [("ybst", s)], scale=psc[:, pc:pc + 1])
                    dma(yb_d[:, pc, tbs], ybst[s], [("ybst", s)], ["yb_d"])
            P.barrier(scr)
            if stop_after == "B3":
                break

            A.reset(PERSIST)
            TWO_PI = 6.28318
            p8 = {n: A.alloc([128, 8], F32) for n in ("lr", "li", "ldt", "dt", "tmp", "mag", "th", "v8", "sth", "cth", "ar", "ai",
                                                      "den", "nr", "zr", "zi", "t1", "t2")}
            pi8 = A.alloc([128, 8], I32)
            scr512 = [A.alloc([128, 512], F32) for _ in range(3)]
            scri = A.alloc([128, 512], I32)
            cosT = A.alloc([128, 8, 512], BF16)
            sinT = A.alloc([128, 8, 512], BF16)
            rhoT = A.alloc([128, 8, 512], F32)
            br_ = A.alloc([128, 8, 16], F32)
            bi_ = A.alloc([128, 8, 16], F32)
            bt = [A.alloc([128, 8, 16], F32) for _ in range(4)]
            Bb = [A.alloc([128, 8, 16], F32) for _ in range(2)]
            Bsrc = A.alloc([128, 2, 8, 128], F32)
            Csrc = A.alloc([128, 2, 8, 128], F32)
            LB = A.alloc([128, 2, 8, 128], BF16)
            LC = A.alloc([128, 2, 8, 128], BF16)
            Dcol = A.alloc([128, 2], F32)
            bglu = A.alloc([128, 2], F32)
            wgs = A.alloc([128, 2, 256], F32)
            wgl = A.alloc([128, 2, 256], BF16)
            hrp = A.alloc([128, 8], F32)
            hip = A.alloc([128, 8], F32)
            NWK = 14
            wk = [[A.alloc([128, 512], BF16) for _ in range(NWK)] for _ in range(2)]
            ypre = [A.alloc([128, 512], F32) for _ in range(2)]
            ygT = A.alloc([128, 2, 512], BF16)
            sgb = [A.alloc([128, 512], F32) for _ in range(2)]
            ycst = [A.alloc([128, 512], BF16) for _ in range(2)]

            def sincos(out_s, out_c, vt, rtag, wtag_s, wtag_c, shape_i, shape_f):
                fi, f1, f2 = shape_i, shape_f[0], shape_f[1]
                for which, outp, wtag in ((0, out_s, wtag_s), (1, out_c, wtag_c)):
                    if which == 1:
                        ts("dve", f2, vt, 0.25, None, ALU.add, None, [rtag], [("sc_f2",)])
                        src, srct = f2, ("sc_f2",)
                    else:
                        src, srct = vt, rtag
                    cp("dve", fi, src, [srct], [("sc_i",)])
                    cp("dve", f1, fi, [("sc_i",)], [("sc_f1",)])
                    tt("dve", f1, src, f1, ALU.subtract, [srct, ("sc_f1",)], [("sc_f1",)])
                    actf(outp, f1, AF.Sin, [("sc_f1",)], [wtag], scale=TWO_PI)

            for gg in range(2):
                hs = slice(gg * 64, gg * 64 + 64)
                dma_slow(p8["lr"][hs, :], lam_re[l].rearrange("(i gg) p -> gg p i", gg=2)[gg], w=["lr"])
                dma_slow(p8["li"][hs, :], lam_im[l].rearrange("(i gg) p -> gg p i", gg=2)[gg], w=["li"])
                dma_slow(p8["ldt"][hs, :], log_dt[l][gg::2].partition_broadcast(64), w=["ldt"])
                dma(br_[hs], b_re[l].rearrange("(i gg) p h -> gg p i h", gg=2)[gg], w=["br"])
                dma(bi_[hs], b_im[l].rearrange("(i gg) p h -> gg p i h", gg=2)[gg], w=["bi"])
            actf(p8["dt"], p8["ldt"], AF.Exp, ["ldt"], ["dt"])
            tt("dve", p8["tmp"], p8["lr"], p8["dt"], ALU.mult, ["lr", "dt"], ["tmp8"])
            actf(p8["mag"], p8["tmp"], AF.Exp, ["tmp8"], ["mag"])
            tt("dve", p8["th"], p8["li"], p8["dt"], ALU.mult, ["li", "dt"], ["th"])
            ts("dve", p8["v8"], p8["th"], 1.0 / (2.0 * math.pi), None, ALU.mult, None, ["th"], ["v8"])
            sincos(p8["sth"], p8["cth"], p8["v8"], "v8", "sth", "cth", pi8, (p8["t1"], p8["t2"]))
            tt("dve", p8["ar"], p8["mag"], p8["cth"], ALU.mult, ["mag", "cth"], ["ar"])
            tt("dve", p8["ai"], p8["mag"], p8["sth"], ALU.mult, ["mag", "sth"], ["ai"])
            tt("dve", p8["den"], p8["lr"], p8["lr"], ALU.mult, ["lr"], ["den"])
            tt("dve", p8["t1"], p8["li"], p8["li"], ALU.mult, ["li", ("sc_f1",)], ["t1"])
            tt("dve", p8["den"], p8["den"], p8["t1"], ALU.add, ["den", "t1"], ["den"])
            P.dve(lambda e: e.reciprocal(out=p8["den"], in_=p8["den"]), ["den"], ["den"])
            ts("dve", p8["nr"], p8["ar"], -1.0, None, ALU.add, None, ["ar"], ["nr"])
            tt("dve", p8["t1"], p8["nr"], p8["lr"], ALU.mult, ["nr", "lr", "t1"], ["t1"])
            tt("dve", p8["t2"], p8["ai"], p8["li"], ALU.mult, ["ai", "li", ("sc_f2",)], ["t2"])
            tt("dve", p8["zr"], p8["t1"], p8["t2"], ALU.add, ["t1", "t2"], ["zr"])
            tt("dve", p8["zr"], p8["zr"], p8["den"], ALU.mult, ["zr", "den"], ["zr"])
            tt("dve", p8["t1"], p8["ai"], p8["lr"], ALU.mult, ["ai", "lr", "t1", "zr"], ["t1"])
            tt("dve", p8["t2"], p8["nr"], p8["li"], ALU.mult, ["nr", "li", "t2", "zr"], ["t2"])
            tt("dve", p8["zi"], p8["t1"], p8["t2"], ALU.subtract, ["t1", "t2"], ["zi"])
            tt("dve", p8["zi"], p8["zi"], p8["den"], ALU.mult, ["zi", "den"], ["zi"])
            for i in range(8):
                ts("dve", scr512[0], iota512, p8["v8"][:, i:i + 1], None, ALU.mult, None, ["iota512", "v8", ("tab", i - 1)], [("vt",)])
                sincos(sinT[:, i, :], cosT[:, i, :], scr512[0], ("vt",), ("sinT", i), ("cosT", i), scri, (scr512[1], scr512[2]))
                ts("pool", rhoT[:, i, :], onesf, p8["mag"][:, i:i + 1], None, ALU.mult, None, ["onesf", "mag", ("sinT", i), ("cosT", i)], [("rho", i), ("tab", i)])
            tabr = [("sinT", i) for i in range(8)] + [("cosT", i) for i in range(8)]
            zrb = p8["zr"].unsqueeze(2).to_broadcast([128, 8, 16])
            zib = p8["zi"].unsqueeze(2).to_broadcast([128, 8, 16])
            tt("dve", bt[0], br_, zrb, ALU.mult, ["br", "zr"], ["bt0"])
            tt("dve", bt[1], bi_, zib, ALU.mult, ["bi", "zi"], ["bt1"])
            tt("dve", Bb[0], bt[0], bt[1], ALU.subtract, ["bt0", "bt1"], ["Bb0"])
            tt("dve", bt[2], bi_, zrb, ALU.mult, ["bi", "zr"], ["bt2"])
            tt("dve", bt[3], br_, zib, ALU.mult, ["br", "zi"], ["bt3"])
            tt("dve", Bb[1], bt[2], bt[3], ALU.add, ["bt2", "bt3"], ["Bb1"])
            memset("pool", Bsrc, 0.0, ["Bsrc"])
            memset("pool", Csrc, 0.0, ["Csrc"])
            k_ = 0
            for ri in range(2):
                for i in range(8):
                    for gg in range(2):
                        hs = slice(gg * 64, gg * 64 + 64)
                        c0 = 32 * (i % 4) + 16 * gg
                        cp(("dve", "pool", "act")[k_ % 3], Bsrc[hs, ri, i, c0:c0 + 16], Bb[ri][hs, i, :], [f"Bb{ri}", "Bsrc"], ["Bsrc"])
                        k_ += 1
            for ri in range(2):
                for i0 in (0, 4):
                    pk = nxt(4)
                    for i in range(i0, i0 + 4):
                        tr(PS[pk][:, (i % 4) * 128:(i % 4) * 128 + 128], Bsrc[:, ri, i, :], identf, ["Bsrc", "identf"], [("ps", pk)])
                    cp("act", LB[:, ri, i0:i0 + 4, :], PS[pk][:, :].rearrange("p (a b) -> p a b", a=4), [("ps", pk)], ["LB"])
            csrc_ap = (c_re, c_im)
            for ri in range(2):
                for gg in range(2):
                    hs = slice(gg * 64, gg * 64 + 64)
                    for m in range(4):
                        c0 = 32 * m + 16 * gg
                        for j in range(2):
                            dma_slow(Csrc[hs, ri, m + 4 * j, c0:c0 + 16], csrc_ap[ri][l, 2 * m + gg + 8 * j].rearrange("h p -> p h"),
                                     r=["Csrc"], w=["Csrc"])
            cp("dve", LC[:, 0], Csrc[:, 0], ["Csrc"], ["LC"])
            ts("dve", LC[:, 1], Csrc[:, 1], -1.0, None, ALU.mult, None, ["Csrc"], ["LC"])
            dma_slow(Dcol, ssm_d[l].rearrange("g h -> (g h)").rearrange("(c p) -> p c", p=128), w=["Dcol"])
            dma_slow(bglu, b_glu[l].rearrange("(c p) -> p c", p=128), w=["bglu"])
            dma(wgs, w_glu[l].rearrange("(kc p) n -> p kc n", p=128), w=["wgs"])
            cp("pool", wgl, wgs, ["wgs"], ["wgl"])
            memset("pool", hrp, 0.0, [("hrp", i) for i in range(8)])
            memset("pool", hip, 0.0, [("hip", i) for i in range(8)])
            units = [(tb, i) for tb in range(NB) for i in range(8)]

            def s5_front(u):
                tb, i = units[u]
                tbs = slice(tb * 512, (tb + 1) * 512)
                ch = i // 4
                s = u % 2
                W = wk[s]
                wt = lambda n: ("wk", s, n)
                pxr = 2 * s
                pxi = 2 * s + 1
                us = usT[:, ch, tbs]
                mm(PS[pxr][:, :], LB[:, 0, i, :], us, True, True, ["LB", ("usT", ch)], [("ps", pxr)])
                mm(PS[pxi][:, :], LB[:, 1, i, :], us, True, True, ["LB", ("usT", ch)], [("ps", pxi)])
                cT_, sT_ = cosT[:, i, :], sinT[:, i, :]
                tt("dve", W[0], PS[pxr][:, :], cT_, ALU.mult, [("ps", pxr)] + tabr, [wt(0)])
                tt("dve", W[1], PS[pxi][:, :], sT_, ALU.mult, [("ps", pxi)] + tabr, [wt(1)])
                tt("pool", W[4], W[0], W[1], ALU.add, [wt(0), wt(1)], [wt(4)])
                tt("dve", W[2], PS[pxi][:, :], cT_, ALU.mult, [("ps", pxi)] + tabr, [wt(2)])
                tt("dve", W[3], PS[pxr][:, :], sT_, ALU.mult, [("ps", pxr)] + tabr, [wt(3)])
                tt("pool", W[5], W[2], W[3], ALU.subtract, [wt(2), wt(3)], [wt(5)])
                P.dve(lambda e, W=W, i=i: e.tensor_tensor_scan(out=W[6], data0=rhoT[:, i, :], data1=W[4], initial=hrp[:, i:i + 1],
                                                               op0=ALU.mult, op1=ALU.add), [wt(4), ("rho", i), ("hrp", i)], [wt(6)])
                P.dve(lambda e, W=W, i=i: e.tensor_tensor_scan(out=W[7], data0=rhoT[:, i, :], data1=W[5], initial=hip[:, i:i + 1],
                                                               op0=ALU.mult, op1=ALU.add), [wt(5), ("rho", i), ("hip", i)], [wt(7)])
                tt("pool", W[8], W[6], cT_, ALU.mult, [wt(6)] + tabr, [wt(8)])
                tt("pool", W[9], W[7], sT_, ALU.mult, [wt(7)] + tabr, [wt(9)])
                tt("pool", W[12], W[8], W[9], ALU.subtract, [wt(8), wt(9)], [wt(12)])
                tt("dve", W[10], W[7], cT_, ALU.mult, [wt(7)] + tabr, [wt(10)])
                tt("dve", W[11], W[6], sT_, ALU.mult, [wt(6)] + tabr, [wt(11)])
                tt("pool", W[13], W[10], W[11], ALU.add, [wt(10), wt(11)], [wt(13)])
                cp("act", hrp[:, i:i + 1], W[12][:, 511:512], [wt(12)], [("hrp", i)])
                cp("act", hip[:, i:i + 1], W[13][:, 511:512], [wt(13)], [("hip", i)])

            def s5_back(u):
                tb, i = units[u]
                tbs = slice(tb * 512, (tb + 1) * 512)
                ch = i // 4
                s = u % 2
                W = wk[s]
                wt = lambda n: ("wk", s, n)
                us = usT[:, ch, tbs]
                py = 4 + ch
                mm(PS[py][:, :], LC[:, 0, i, :], W[12], i % 4 == 0, False, ["LC", wt(12)], [("ps", py)])
                mm(PS[py][:, :], LC[:, 1, i, :], W[13], False, i % 4 == 3, ["LC", wt(13)], [("ps", py)])
                if i % 4 == 3:
                    stt("dve", ypre[ch], us, Dcol[:, ch:ch + 1], PS[py][:, :], ALU.mult, ALU.add, [("usT", ch), "Dcol", ("ps", py)], [("ypre", ch)])
                    actf(ygT[:, ch, :], ypre[ch], AF.Gelu_apprx_tanh, [("ypre", ch)], [("ygT", ch)])
                if i == 7:
                    for oc in range(2):
                        pz = 6 + oc
                        for kc in range(2):
                            mm(PS[pz][:, :], wgl[:, kc, oc * 128:(oc + 1) * 128], ygT[:, kc, :], kc == 0, kc == 1,
                               ["wgl", ("ygT", 0), ("ygT", 1)], [("ps", pz)])
                        actf(sgb[oc], PS[pz][:, :], AF.Sigmoid, [("ps", pz), "bglu"], [("sgb", oc)], bias=bglu[:, oc:oc + 1], scale=1.0)
                        tt("pool", ycst[oc], ygT[:, oc, :], sgb[oc], ALU.mult, [("ygT", oc), ("sgb", oc)], [("ycst", oc)])
                        dma(yc_d[:, oc, tbs], ycst[oc], [("ycst", oc)], ["yc_d"])

            for u in range(len(units) + 1):
                if u < len(units):
                    s5_front(u)
                if u >= 1:
                    s5_back(u - 1)
            P.barrier(scr)
            if stop_after == "B4":
                break
            A.reset(PERSIST_C)
            wg = A.alloc([128, 8, 3072], BF16)
            wbr = A.alloc([128, 8, D], BF16)
            wo = A.alloc([128, 8, D], BF16)
            wrs = A.alloc([128, 8, NE], F32)
            wrb = A.alloc([128, 8, NE], BF16)
            gB = A.alloc([128, D], F32)
            bB = A.alloc([128, D], F32)
            brB = A.alloc([128, NE], F32)
            xTc = [A.alloc([128, 8, 512], BF16) for _ in range(2)]
            ybk = [A.alloc([128, 8, 512], BF16) for _ in range(2)]
            mrg = A.alloc([128, 8, 512], BF16)
            sgt = [A.alloc([128, 512], F32) for _ in range(2)]
            macc = [A.alloc([128, 512], F32) for _ in range(2)]
            mtmp = [A.alloc([128, 512], F32) for _ in range(2)]
            xw = [A.alloc([128, D], F32) for _ in range(2)]
            x1b = [A.alloc([128, D], BF16) for _ in range(2)]
            x1T = A.alloc([128, 8, 128], BF16)
            sm = {n: A.alloc([128, NE], F32) for n in ("lg", "maskf", "ex", "exm", "valid", "d0", "d1", "Wd", "junk", "junk2")}
            mx8 = A.alloc([128, 8], F32)
            sm1 = {n: A.alloc([128, 1], F32) for n in ("nmx", "ssum", "rsum", "rs", "nmr")}
            dsel = A.alloc([128, 4], F32)
            bst = A.alloc([128, 2, 6], F32)
            mv = A.alloc([128, 2], F32)

            def wload(dst_ap, src_ap, wtag):
                dma(dst_ap, src_ap.rearrange("(kc p) n -> p kc n", p=128), w=[wtag], q="pool")

            for b in range(3):
                for half in range(2):
                    c0 = 2056 + b * 1024 + half * 512
                    wload(wg[:, :, b * 1024 + half * 512:b * 1024 + half * 512 + 512], w_in[l][:, c0:c0 + 512], "wg")
            for half in range(2):
                hs = slice(half * 512, half * 512 + 512)
                wload(wbr[:, 0:4, hs], w_br[0][l][:, hs], "wbr")
                wload(wbr[:, 4:6, hs], w_br[1][l][:, hs], "wbr")
                wload(wbr[:, 6:8, hs], w_br[2][l][:, hs], "wbr")
                wload(wo[:, :, hs], w_out[l][:, hs], "wo")
            dma(wrs, w_router[l].rearrange("(kc p) n -> p kc n", p=128), w=["wrs"])
            cp("dve", wrb, wrs, ["wrs"], ["wrb"])

            def load_ln(which, gB_, bB_):
                dma(gB_, ln_g[which][l].partition_broadcast(128), w=["gB"])
                dma(bB_, ln_b[which][l].partition_broadcast(128), w=["bB"])

            def layernorm(xw_ap, xtag, gB_, bB_, bst_, mv_, rs_, nmr_):
                for half in range(2):
                    P.dve(lambda e, half=half: e.bn_stats(out=bst_[:, half, :], in_=xw_ap[:, half * 512:(half + 1) * 512]), [xtag], ["bst"])
                P.dve(lambda e: e.bn_aggr(out=mv_, in_=bst_[:, :, :].rearrange("p a b -> p (a b)")), ["bst"], ["mv"])
                actf(rs_, mv_[:, 1:2], AF.Sqrt, ["mv", "eps_c"], ["rs"], bias=eps_c[:, 0:1], scale=1.0)
                P.dve(lambda e: e.reciprocal(out=rs_, in_=rs_), ["rs"], ["rs"])
                stt("dve", nmr_, mv_[:, 0:1], -1.0, rs_, ALU.mult, ALU.mult, ["mv", "rs"], ["nmr"])
                actf(xw_ap, xw_ap, AF.Identity, [xtag, "rs", "nmr"], [xtag], bias=nmr_[:, 0:1], scale=rs_[:, 0:1])
                tt("pool", xw_ap, xw_ap, gB_, ALU.mult, [xtag, "gB"], [xtag])
                tt("pool", xw_ap, xw_ap, bB_, ALU.add, [xtag, "bB"], [xtag])

            load_ln(0, gB, bB)
            dma(brB, b_router[l].partition_broadcast(128), w=["brB"])
            kq = 0
            for tb in range(NB):
                tbs = slice(tb * 512, (tb + 1) * 512)
                s = tb % 2
                dma(xTc[s], xT_d[:, :, tbs], ["xT_d"], [("xTc", s)])
                dma(ybk[s][:, 0:4, :], ya_d[:, :, tbs], ["ya_d"], [("ybk", s)])
                dma(ybk[s][:, 4:6, :], yb_d[:, :, tbs], ["yb_d"], [("ybk", s)])
                dma(ybk[s][:, 6:8, :], yc_d[:, :, tbs], ["yc_d"], [("ybk", s)])
                for c in range(8):
                    cs_ = slice(c * 128, (c + 1) * 128)
                    ma = macc[c % 2]
                    for b in range(3):
                        q2 = kq % 2
                        kq += 1
                        pg, pm = 2 * q2, 2 * q2 + 1
                        for kc in range(8):
                            mm(PS[pg][:, :], wg[:, kc, b * 1024 + c * 128:b * 1024 + c * 128 + 128], xTc[s][:, kc, :], kc == 0, kc == 7,
                               ["wg", ("xTc", s)], [("ps", pg)])
                        kcs = ((0, 1, 2, 3), (4, 5), (6, 7))[b]
                        for kc in kcs:
                            mm(PS[pm][:, :], wbr[:, kc, cs_], ybk[s][:, kc, :], kc == kcs[0], kc == kcs[-1], ["wbr", ("ybk", s)], [("ps", pm)])
                        actf(sgt[q2], PS[pg][:, :], AF.Sigmoid, [("ps", pg)], [("sgt", q2)])
                        if b == 0:
                            tt("dve", ma, PS[pm][:, :], sgt[q2], ALU.mult, [("ps", pm), ("sgt", q2)], [("macc", c % 2)])
                        else:
                            tt("dve", mtmp[b - 1], PS[pm][:, :], sgt[q2], ALU.mult, [("ps", pm), ("sgt", q2)], [("mtmp", b - 1)])
                            if b == 1:
                                tt("pool", ma, ma, mtmp[0], ALU.add, [("macc", c % 2), ("mtmp", 0)], [("macc", c % 2)])
                            else:
                                tt("pool", mrg[:, c, :], ma, mtmp[1], ALU.add, [("macc", c % 2), ("mtmp", 1)], [("mrg", c)])
                mrg_r = [("mrg", c) for c in range(8)]
                def c_part1(ti, tb=tb, mrg_r=mrg_r):
                    i = tb * 4 + ti
                    s1 = i % 2
                    xt_ = ("xw", s1)
                    dma(xw[s1], xsrc[i * 128:(i + 1) * 128, :], w=[xt_])
                    for half in range(2):
                        pk = 6 + half
                        for kc in range(8):
                            mm(PS[pk][:, :], mrg[:, kc, ti * 128:(ti + 1) * 128], wo[:, kc, half * 512:(half + 1) * 512], kc == 0, kc == 7,
                               mrg_r + ["wo"], [("ps", pk)])
                        hs = slice(half * 512, (half + 1) * 512)
                        stt("dve", xw[s1][:, hs], xw[s1][:, hs], ALPHA, PS[pk][:, :], ALU.mult, ALU.add, [xt_, ("ps", pk)], [xt_])
                    layernorm(xw[s1], xt_, gB, bB, bst, mv, sm1["rs"], sm1["nmr"])
                    dma(x1_d[i * 128:(i + 1) * 128, :], xw[s1], [xt_], ["x1_d"])
                    cp("act", x1b[s1], xw[s1], [xt_], [("x1b", s1)])

                def c_part2(ti, tb=tb):
                    i = tb * 4 + ti
                    s1 = i % 2
                    xt_ = ("xw", s1)
                    for kc in range(8):
                        tr(psb(4)[:, kc, :], x1b[s1][:, kc * 128:(kc + 1) * 128], identb, [("x1b", s1), "identb"], [("ps", 4)])
                    cp("act", x1T, psb(4), [("ps", 4)], ["x1T"])
                    for kc in range(8):
                        mm(PS[5][:, 0:NE], x1T[:, kc, :], wrb[:, kc, :], kc == 0, kc == 7, ["x1T", "wrb"], [("ps5", "lg")])
                    tt("dve", sm["lg"], PS[5][:, 0:NE], brB, ALU.add, [("ps5", "lg"), "brB"], ["lg"])
                    P.dve(lambda e: e.max(out=mx8, in_=sm["lg"]), ["lg"], ["mx8"])
                    ts("dve", sm["maskf"], sm["lg"], mx8[:, 3:4], None, ALU.is_ge, None, ["lg", "mx8"], ["maskf"])
                    cp("pool", masks[:, i, :], sm["maskf"], ["maskf"], [("masks", i)])
                    ts("dve", sm1["nmx"], mx8[:, 0:1], -1.0, None, ALU.mult, None, ["mx8"], ["nmx"])
                    actf(sm["ex"], sm["lg"], AF.Exp, ["lg", "nmx"], ["ex"], bias=sm1["nmx"][:, 0:1], scale=1.0)
                    stt("dve", sm["exm"], sm["ex"], 1.0, sm["maskf"], ALU.mult, ALU.mult, ["ex", "maskf"], ["exm", "ssum"], accum=sm1["ssum"][:, 0:1])
                    P.dve(lambda e: e.reciprocal(out=sm1["rsum"], in_=sm1["ssum"]), ["ssum"], ["rsum"])
                    mm(PS[5][:, 64:64 + NE], lstrict, masks[:, i, :], True, i == 0, ["lstrict", ("masks", i)], [("ps5", "pos")])
                    for j in range(i):
                        mm(PS[5][:, 64:64 + NE], onesb, masks[:, j, :], False, j == i - 1, ["onesb", ("masks", j)], [("ps5", "pos")])
                    ts("dve", sm["valid"], PS[5][:, 64:64 + NE], float(CAP), None, ALU.is_lt, None, [("ps5", "pos")], ["valid"])
                    tt("dve", sm["d0"], PS[5][:, 64:64 + NE], eC, ALU.add, [("ps5", "pos"), "eC"], ["d0"])
                    stt("dve", sm["d1"], sm["d0"], float(DUMMY), sm["valid"], ALU.subtract, ALU.mult, ["d0", "valid"], ["d1"])
                    stt("dve", sm["Wd"], sm["exm"], sm1["rsum"][:, 0:1], sm["valid"], ALU.mult, ALU.mult, ["exm", "rsum", "valid"], ["Wd"])
                    for k in range(4):
                        stt("dve", sm["junk"], sm["lg"], mx8[:, k:k + 1], sm["d1"], ALU.is_equal, ALU.mult, ["lg", "mx8", "d1"], ["junk", "dsel"],
                            accum=dsel[:, k:k + 1])
                        stt("dve", sm["junk2"], sm["lg"], mx8[:, k:k + 1], sm["Wd"], ALU.is_equal, ALU.mult, ["lg", "mx8", "Wd"],
                            ["junk2", ("wsels", i)], accum=wsels[:, i, k:k + 1])
                    ts("dve", idxs[:, i, :], dsel, float(DUMMY), None, ALU.add, None, ["dsel"], [("idxs", i)])
                    tr(PS[5][0:NE, 128:256], sm["Wd"], identf, ["Wd", "identf"], [("ps5", "wdt")])
                    cp("act", WdTs[:, i, :], PS[5][0:NE, 128:256], [("ps5", "wdt")], [("WdTs", i)])
                    for k in range(4):
                        P.dma(lambda e, i=i, k=k, s1=s1: e.indirect_dma_start(
                            out=xg_d, out_offset=bass.IndirectOffsetOnAxis(ap=idxs[:, i, k:k + 1], axis=0), in_=x1b[s1], in_offset=None),
                            [("x1b", s1), ("idxs", i)], ["xg_d"], q="pool")

                for tp_ in range(0, 4, 2):
                    c_part1(tp_)
                    c_part1(tp_ + 1)
                    c_part2(tp_)
                    c_part2(tp_ + 1)
            P.barrier(scr)
            if stop_after == "C":
                break

            A.reset(PERSIST_C)
            bupA = A.alloc([128, NE, 8, 2], F32)
            wu = [A.alloc([128, 8, 512], BF16) for _ in range(8)]
            wdn = [A.alloc([128, 8, 512], BF16) for _ in range(4)]
            xgl = A.alloc([128, CT, D], BF16)
            xgT = [A.alloc([128, 8, CAP], BF16) for _ in range(2)]
            actT = A.alloc([128, 8, CAP], BF16)
            yst2 = [A.alloc([128, 512], F32) for _ in range(6)]
            ET = {n: [A.alloc([128, 512], F32) for _ in range(3)] for n in ("g", "s", "l", "gs")}
            for e4 in range(0, NE, 4):
                dma_slow(bupA[:, e4:e4 + 4], b_up[l, e4:e4 + 4].rearrange("e (c p two) -> p e c two", p=128, two=2), w=["bupA"])
            ts("dve", bupA[:, :, :, 1:2], bupA[:, :, :, 1:2], 1.0, None, ALU.add, None, ["bupA"], ["bupA"])
            eD = [0]

            def load_up(ex, blk):
                sl = (ex % 2) * 4 + blk
                dma(wu[sl], w_up[l, ex][:, blk * 512:(blk + 1) * 512].rearrange("(kc p) n -> p kc n", p=128), w=[("wu", sl)], q="pool")

            def load_dn(ex, half):
                sl = (ex % 2) * 2 + half
                dma(wdn[sl], w_down[l, ex][:, half * 512:(half + 1) * 512].rearrange("(kc p) n -> p kc n", p=128), w=[("wdn", sl)], q="pool")

            def load_tok(ex):
                dma(xgl, xg_d[ex * CAP:(ex + 1) * CAP, :].rearrange("(i p) d -> p i d", p=128), ["xg_d"], ["xgl"])

            def transposes(ex):
                xs_ = ex % 2
                for it in range(CT):
                    pk = 6 + (it % 2)
                    for kc in range(8):
                        tr(psb(pk)[:, kc, :], xgl[:, it, kc * 128:(kc + 1) * 128], identb, ["xgl", "identb"], [("ps", pk)])
                    cp("act" if it % 2 == 0 else "dve", xgT[xs_][:, :, it * 128:(it + 1) * 128], psb(pk), [("ps", pk)], [("xgT", xs_)])

            def up_block(ex, blk):
                xs_ = ex % 2
                for fc in range(2):
                    ffc = blk * 2 + fc
                    for (n0, n) in ((0, 512), (512, CAP - 512)):
                        q2 = eD[0] % 3
                        eD[0] += 1
                        pg, pl = 2 * q2, 2 * q2 + 1
                        for gl, pk in ((0, pg), (1, pl)):
                            for kc in range(8):
                                mm(PS[pk][:, 0:n], wu[(ex % 2) * 4 + blk][:, kc, fc * 256 + gl:fc * 256 + 256:2], xgT[xs_][:, kc, n0:n0 + n], kc == 0, kc == 7,
                                   [("wu", (ex % 2) * 4 + blk), ("xgT", xs_)], [("ps", pk)])
                        g, sg_, l_, gs = (ET[nm][q2][:, 0:n] for nm in ("g", "s", "l", "gs"))
                        et = lambda nm: ("ET", nm, q2)
                        ts("dve", g, PS[pg][:, 0:n], bupA[:, ex, ffc, 0:1], 7.0, ALU.add, ALU.min, [("ps", pg), "bupA"], [et("g")])
                        actf(sg_, g, AF.Sigmoid, [et("g")], [et("s")], scale=1.702)
                        ts("dve", l_, PS[pl][:, 0:n], bupA[:, ex, ffc, 1:2], 8.0, ALU.add, ALU.min, [("ps", pl), "bupA"], [et("l")])
                        tt("pool", gs, g, sg_, ALU.mult, [et("g"), et("s")], [et("gs")])
                        stt("dve", actT[:, ffc, n0:n0 + n], l_, -6.0, gs, ALU.max, ALU.mult, [et("gs"), et("l")], [("actT", ffc)])

            def down_half(ex, half):
                actT_r = [("actT", f) for f in range(8)]
                for it in range(CT):
                    ys = it % 6
                    pk = 6 + (it % 2)
                    for kc in range(8):
                        mm(PS[pk][:, :], actT[:, kc, it * 128:(it + 1) * 128], wdn[(ex % 2) * 2 + half][:, kc, :], kc == 0, kc == 7,
                           actT_r + [("wdn", (ex % 2) * 2 + half)], [("ps", pk)])
                    cp("act", yst2[ys][:, 0:512], PS[pk][:, :], [("ps", pk)], [("yst2", ys)])
                    dma(yg_d[ex * CAP + it * 128:ex * CAP + (it + 1) * 128, half * 512:(half + 1) * 512], yst2[ys][:, 0:512],
                        [("yst2", ys)], ["yg_d"], q="act")

            for blk in range(4):
                load_up(0, blk)
            for half in range(2):
                load_dn(0, half)
            load_tok(0)
            transposes(0)
            for ex in range(NE):
                nx = ex + 1 < NE
                if nx:
                    load_tok(ex + 1)
                for blk in range(4):
                    if nx:
                        load_up(ex + 1, blk)
                    up_block(ex, blk)
                if nx:
                    transposes(ex + 1)
                for half in range(2):
                    if nx:
                        load_dn(ex + 1, half)
                    down_half(ex, half)
            P.barrier(scr)
            if stop_after == "D":
                break

            A.reset(PERSIST_C)
            gB2 = A.alloc([128, D], F32)
            bB2 = A.alloc([128, D], F32)
            bdn = A.alloc([32, D], F32)
            xw2 = [A.alloc([128, D], F32) for _ in range(2)]
            yk = [[A.alloc([128, D], F32) for _ in range(4)] for _ in range(2)]
            accB = [A.alloc([128, D], F32) for _ in range(2)]
            sm1e = {n: A.alloc([128, 1], F32) for n in ("rs", "nmr")}
            bste = A.alloc([128, 2, 6], F32)
            mve = A.alloc([128, 2], F32)
            load_ln(1, gB2, bB2)
            dma(bdn, b_down[l], w=["bdn"])
            def e_front(i):
                s1 = i % 2
                dma(xw2[s1], x1_d[i * 128:(i + 1) * 128, :], ["x1_d"], [("xw", s1)])
                for k in range(4):
                    P.dma(lambda e, i=i, k=k, s1=s1: e.indirect_dma_start(
                        out=yk[s1][k], out_offset=None, in_=yg_d, in_offset=bass.IndirectOffsetOnAxis(ap=idxs[:, i, k:k + 1], axis=0)),
                        ["yg_d", ("idxs", i)], [("yk", s1, k)], q="pool")

            def e_back(i):
                s1 = i % 2
                xt_ = ("xw", s1)
                for half in range(2):
                    pk = 2 * s1 + half
                    hs = slice(half * 512, (half + 1) * 512)
                    mm(PS[pk][:, :], WdTs[:, i, :], bdn[:, hs], True, True, [("WdTs", i), "bdn"], [("ps", pk)])
                    stt("dve", xw2[s1][:, hs], xw2[s1][:, hs], ALPHA, PS[pk][:, :], ALU.mult, ALU.add, [xt_, ("ps", pk)], [xt_])
                actf(accB[s1], yk[s1][2], AF.Copy, [("yk", s1, 2), ("wsels", i)], [("accB", s1)], scale=wsels[:, i, 2:3])
                stt("dve", xw2[s1], yk[s1][0], wsels[:, i, 0:1], xw2[s1], ALU.mult, ALU.add, [("yk", s1, 0), ("wsels", i), xt_], [xt_])
                stt("dve", xw2[s1], yk[s1][1], wsels[:, i, 1:2], xw2[s1], ALU.mult, ALU.add, [("yk", s1, 1), ("wsels", i), xt_], [xt_])
                stt("dve", accB[s1], yk[s1][3], wsels[:, i, 3:4], accB[s1], ALU.mult, ALU.add, [("yk", s1, 3), ("wsels", i), ("accB", s1)], [("accB", s1)])
                tt("pool", xw2[s1], xw2[s1], accB[s1], ALU.add, [xt_, ("accB", s1)], [xt_])
                layernorm(xw2[s1], xt_, gB2, bB2, bste, mve, sm1e["rs"], sm1e["nmr"])
                o_ = dma(xdst[i * 128:(i + 1) * 128, :], xw2[s1], [xt_], ["xs_out"])
                if l == n_layers - 1:
                    fin_ops.append(o_)

            for i in range(NT + 1):
                if i < NT:
                    e_front(i)
                if i >= 1:
                    e_back(i - 1)
            P.barrier(scr)
        P.emit(final_wait_ops=fin_ops)
    return nc


def kernel(**inputs):
    x = np.asarray(inputs["x"], np.float32)
    nb = x.shape[0]
    nc = build_nc(NL, dbg=False)
    shared = {k: np.ascontiguousarray(np.asarray(v, np.float32)) for k, v in inputs.items() if k != "x"}
    in_maps = [dict(shared, x=np.ascontiguousarray(x[b])) for b in range(nb)]
    res = run_bass_kernel_spmd(nc, in_maps, core_ids=list(range(nb)))
    return np.stack([np.asarray(r["out"], np.float32) for r in res.results], axis=0)
```

```python
import math
from contextlib import ExitStack

import numpy as np
import concourse.bass as bass
import concourse.mybir as mybir
from concourse.bass_utils import run_bass_kernel_spmd

F32 = mybir.dt.float32
BF16 = mybir.dt.bfloat16
I32 = mybir.dt.int32
AF = mybir.ActivationFunctionType
ALU = mybir.AluOpType

D = 1024
T = 4096
NL = 4
NT = T // 128
NB = T // 512
N_IN = 5128
NE = 32
CAP = 768
CT = CAP // 128
DUMMY = NE * CAP
ALPHA = (2.0 * NL) ** 0.25
LN_EPS = 1e-5
NEG = -30000.0

ENGS = ("pe", "act", "dve", "pool", "sp")


class Op:
    __slots__ = ("id", "eng", "fn", "deps", "dma", "need_sig", "sig", "is_mm")

    def __init__(self, id, eng, fn, deps, dma, is_mm):
        self.id = id
        self.eng = eng
        self.fn = fn
        self.deps = deps
        self.dma = dma
        self.need_sig = dma
        self.sig = None
        self.is_mm = is_mm


class Prog:
    def __init__(self, nc):
        self.nc = nc
        self.ops = []
        self.last_write = {}
        self.readers = {}
        self.n_dma_sems = {"sp": 8, "pool": 12, "act": 12}
        self.n_eng_sems = 4
        self.open_dmas = []
        self.nbar = 0

    def op(self, eng, fn, reads=(), writes=(), dma=False, mm=False, extra=()):
        deps = set(extra)
        for r in reads:
            w = self.last_write.get(r)
            if w is not None:
                deps.add(w)
        for w_ in writes:
            w = self.last_write.get(w_)
            if w is not None:
                deps.add(w)
            rd = self.readers.get(w_)
            if rd is not None:
                deps.update(rd[0].values())
                deps.update(rd[1])
        oid = len(self.ops)
        o = Op(oid, eng, fn, sorted(deps), dma, mm)
        self.ops.append(o)
        for d in o.deps:
            self.ops[d].need_sig = True
        for r in reads:
            rd = self.readers.setdefault(r, ({}, []))
            if dma:
                rd[1].append(oid)
            else:
                rd[0][eng] = oid
        for w_ in writes:
            self.last_write[w_] = oid
            self.readers[w_] = ({}, [])
        if dma:
            self.open_dmas.append(oid)
        return oid

    def pe(self, fn, reads=(), writes=()):
        return self.op("pe", fn, reads, writes, mm=True)

    def act(self, fn, reads=(), writes=()):
        return self.op("act", fn, reads, writes)

    def dve(self, fn, reads=(), writes=()):
        return self.op("dve", fn, reads, writes)

    def pool(self, fn, reads=(), writes=()):
        return self.op("pool", fn, reads, writes)

    def dma(self, fn, reads=(), writes=(), q="sp"):
        return self.op(q, fn, reads, writes, dma=True)

    def barrier(self, scr):
        k = self.nbar
        self.nbar += 1
        dm = list(self.open_dmas)
        self.open_dmas = []
        tags = []
        for e in ("pe", "act", "dve", "pool", "sp"):
            tg = ("bar", k, e)
            tags.append(tg)
            if e == "pe":
                self.op("pe", lambda en: en.matmul(scr["ps"], lhsT=scr["idb"][:, 0:2], rhs=scr["idb"][:, 0:2], start=True, stop=True),
                        writes=[tg], extra=dm)
            elif e == "act":
                self.op("act", lambda en: en.copy(out=scr["a"], in_=scr["c"]), writes=[tg], extra=dm)
            elif e == "dve":
                self.op("dve", lambda en: en.tensor_copy(out=scr["v"], in_=scr["c"]), writes=[tg], extra=dm)
            elif e == "pool":
                self.op("pool", lambda en: en.tensor_copy(out=scr["g"], in_=scr["c"]), writes=[tg], extra=dm)
            else:
                self.op("sp", lambda en: en.dma_start(out=scr["d1"], in_=scr["d0"]), writes=[tg], dma=True, extra=dm)
        self.open_dmas = []
        for e in ("pe", "act", "dve", "pool"):
            tg2 = ("bar2", k, e)
            if e == "pe":
                self.op("pe", lambda en: en.matmul(scr["ps"], lhsT=scr["idb"][:, 0:2], rhs=scr["idb"][:, 0:2], start=True, stop=True),
                        reads=tags, writes=[tg2])
            elif e == "act":
                self.op("act", lambda en: en.copy(out=scr["a"], in_=scr["c"]), reads=tags, writes=[tg2])
            elif e == "dve":
                self.op("dve", lambda en: en.tensor_copy(out=scr["v"], in_=scr["c"]), reads=tags, writes=[tg2])
            else:
                self.op("pool", lambda en: en.tensor_copy(out=scr["g"], in_=scr["c"]), reads=tags, writes=[tg2])
        self.op("sp", lambda en: en.dma_start(out=scr["d1"], in_=scr["d0"]), reads=tags, writes=[("bar2", k, "sp")], dma=True)

    def emit(self, final_wait_ops=()):
        nc = self.nc
        with ExitStack() as es:
            eng_sems = {e: [es.enter_context(nc.semaphore(f"s_{e}{k}")) for k in range(self.n_eng_sems)]
                        for e in ("pe", "act", "dve", "pool")}
            dma_sems = {q: [es.enter_context(nc.semaphore(f"d_{q}{k}")) for k in range(n)]
                        for q, n in self.n_dma_sems.items()}
            eng_cnt = {e: [0] * self.n_eng_sems for e in eng_sems}
            eng_rr = {e: 0 for e in eng_sems}
            dma_cnt = {q: [0] * n for q, n in self.n_dma_sems.items()}
            dma_rr = {q: 0 for q in dma_sems}
            dma_prev = {}
            for o in self.ops:
                if o.dma:
                    q = o.eng
                    k = dma_rr[q]
                    dma_rr[q] = (k + 1) % len(dma_sems[q])
                    prev = dma_cnt[q][k]
                    dma_cnt[q][k] += 16
                    o.sig = (dma_sems[q][k], dma_cnt[q][k], 16)
                    dma_prev[o.id] = (dma_sems[q][k], prev)
                elif o.need_sig:
                    e = o.eng
                    k = eng_rr[e]
                    eng_rr[e] = (k + 1) % self.n_eng_sems
                    eng_cnt[e][k] += 1
                    o.sig = (eng_sems[e][k], eng_cnt[e][k], 1)
            per_eng = {e: [o for o in self.ops if o.eng == e] for e in ENGS}
            ops = self.ops
            final_wait_ops = list(final_wait_ops)

            def run_engine(ename, eobj):
                waited = {}

                def wait(sem, val):
                    key = id(sem)
                    if waited.get(key, 0) >= val:
                        return
                    eobj.wait_ge(sem, val)
                    waited[key] = val

                for o in per_eng[ename]:
                    need = {}
                    for d in o.deps:
                        do = ops[d]
                        if do.eng == ename and do.is_mm and o.is_mm:
                            continue
                        sem, val, _ = do.sig
                        k_ = id(sem)
                        if k_ not in need or need[k_][1] < val:
                            need[k_] = (sem, val)
                    for sem, val in need.values():
                        wait(sem, val)
                    if o.dma:
                        sem, prev = dma_prev[o.id]
                        if prev > 0:
                            wait(sem, prev)
                    ins = o.fn(eobj)
                    if o.sig is not None:
                        sem, val, inc = o.sig
                        ins.then_inc(sem, inc)
                if ename == "sp":
                    for oid in final_wait_ops:
                        sem, val, _ = ops[oid].sig
                        wait(sem, val)

            with nc.Block() as block:
                @block.sync
                def _(e):
                    run_engine("sp", e)

                @block.tensor
                def _(e):
                    run_engine("pe", e)

                @block.scalar
                def _(e):
                    run_engine("act", e)

                @block.vector
                def _(e):
                    run_engine("dve", e)

                @block.gpsimd
                def _(e):
                    run_engine("pool", e)


class Arena:
    def __init__(self, ap, nwords):
        self.ap = ap
        self.n = nwords
        self.off = 0

    def reset(self, off=0):
        self.off = off

    def alloc(self, shape, dt, parts=None):
        esz = 4 if dt in (F32, I32) else 2
        free = int(np.prod(shape[1:]))
        words = (free * esz + 3) // 4
        words = (words + 7) // 8 * 8
        assert self.off + words <= self.n, f"SBUF arena overflow: need {self.off + words} > {self.n}"
        v = self.ap[0:shape[0], self.off:self.off + words]
        self.off += words
        if dt != F32:
            v = v.bitcast(dt)
        v = v[:, 0:free]
        if len(shape) == 3:
            v = v.rearrange("p (a b) -> p a b", a=shape[1])
        elif len(shape) == 4:
            v = v.rearrange("p (a b c) -> p a b c", a=shape[1], b=shape[2])
        return v


def build_nc(n_layers=NL, dbg=False, stop_after=None):
    nc = bass.Bass("TRN2", target_bir_lowering=False)
    L = NL

    def din(name, shape):
        return nc.dram_tensor(name, list(shape), F32, kind="ExternalInput").ap()

    x_in = din("x", [T, D])
    w_in = din("w_in", [L, D, N_IN])
    b_forget = din("b_forget", [L, 8])
    w_pool = din("w_pool", [L, 4, 64, 64])
    pool_scale = din("pool_scale", [L, 256])
    lam_re = din("ssm_lambda_re", [L, 16, 64])
    lam_im = din("ssm_lambda_im", [L, 16, 64])
    log_dt = din("ssm_log_dt", [L, 16])
    b_re = din("ssm_b_re", [L, 16, 64, 16])
    b_im = din("ssm_b_im", [L, 16, 64, 16])
    c_re = din("ssm_c_re", [L, 16, 16, 64])
    c_im = din("ssm_c_im", [L, 16, 16, 64])
    ssm_d = din("ssm_d", [L, 16, 16])
    w_glu = din("w_glu", [L, 256, 256])
    b_glu = din("b_glu", [L, 256])
    w_br = [din("w_branch_a", [L, 512, D]), din("w_branch_b", [L, 256, D]), din("w_branch_c", [L, 256, D])]
    w_out = din("w_out", [L, D, D])
    ln_g = [din("ln1_g", [L, D]), din("ln2_g", [L, D])]
    ln_b = [din("ln1_b", [L, D]), din("ln2_b", [L, D])]
    w_router = din("w_router", [L, D, NE])
    b_router = din("b_router", [L, NE])
    w_up = din("w_up", [L, NE, D, 2 * D])
    b_up = din("b_up", [L, NE, 2 * D])
    w_down = din("w_down", [L, NE, D, D])
    b_down = din("b_down", [L, NE, D])
    out = nc.dram_tensor("out", [T, D], F32, kind="ExternalOutput").ap()

    skind = "ExternalOutput" if dbg else "Internal"

    def dscr(name, shape, dt):
        return nc.dram_tensor(name, list(shape), dt, kind=skind).ap()

    xs = dscr("xs", [T, D], F32)
    xT_d = dscr("xT_d", [128, 8, T], BF16)
    qT_d = dscr("qT_d", [512, T], BF16)
    kT_d = dscr("kT_d", [512, T], BF16)
    v_d = dscr("v_d", [T, 512], BF16)
    c_d = dscr("c_d", [8, T], F32)
    ya_d = dscr("ya_d", [128, 4, T], BF16)
    yb_d = dscr("yb_d", [128, 2, T], BF16)
    yc_d = dscr("yc_d", [128, 2, T], BF16)
    x1_d = dscr("x1_d", [T, D], F32)
    xg_d = dscr("xg_d", [NE * CAP + 128, D], BF16)
    yg_d = dscr("yg_d", [NE * CAP + 128, D], F32)
    bar_d = nc.dram_tensor("bar_d", [2, 16], F32, kind="Internal").ap()

    P = Prog(nc)
    es = ExitStack()
    with es:
        NW = 52900
        arena_t = es.enter_context(nc.sbuf_tensor("arena", [128, NW], F32))
        misc = es.enter_context(nc.sbuf_tensor("misc", [128, 64], F32))
        PS = [es.enter_context(nc.psum_tensor(f"ps{k}", [128, 512], F32)) for k in range(8)]
        A = Arena(arena_t, NW)

        def psb(k):
            return PS[k][:, :].bitcast(BF16).rearrange("p (a b) -> p a b", a=8)

        identf = A.alloc([128, 128], F32)
        identb = A.alloc([128, 128], BF16)
        onesb = A.alloc([128, 128], BF16)
        lstrict = A.alloc([128, 128], BF16)
        onesf = A.alloc([128, 512], F32)
        eC = A.alloc([128, NE], F32)
        invc = A.alloc([128, 2, 16], F32)
        iota512 = A.alloc([128, 512], F32)
        masks = A.alloc([128, NT, NE], BF16)
        idxs = A.alloc([128, NT, 4], I32)
        wsels = A.alloc([128, NT, 4], F32)
        WdTs = A.alloc([32, NT, 128], F32)
        eps_c = A.alloc([128, 1], F32)
        PERSIST_C = A.off
        fT = A.alloc([8, T], F32)
        upT = A.alloc([128, 2, 16 + T], F32)
        usT = A.alloc([128, 2, T], BF16)
        PERSIST = A.off

        scr = {"ps": PS[7][0:2, 0:2], "idb": identb, "a": misc[0:1, 0:1], "v": misc[0:1, 1:2], "g": misc[0:1, 2:3],
               "c": misc[0:1, 8:9], "d0": bar_d[0:1, :], "d1": bar_d[1:2, :]}

        P.pool(lambda e: e.memset(misc[:, :], 0.0), writes=["misc"])
        P.pool(lambda e: e.memset(onesf, 1.0), writes=["onesf"])
        P.pool(lambda e: e.memset(eps_c, LN_EPS), writes=["eps_c"])
        P.pool(lambda e: e.affine_select(out=identf, in_=onesf[:, 0:128], pattern=[[-1, 128]], compare_op=ALU.is_equal,
                                         fill=0.0, base=0, channel_multiplier=1), reads=["onesf"], writes=["identf"])
        P.dve(lambda e: e.tensor_copy(out=identb, in_=identf), reads=["identf"], writes=["identb"])
        P.dve(lambda e: e.tensor_copy(out=onesb, in_=onesf[:, 0:128]), reads=["onesf"], writes=["onesb"])
        P.pool(lambda e: e.affine_select(out=lstrict, in_=onesb, pattern=[[1, 128]], compare_op=ALU.is_gt,
                                         fill=0.0, base=0, channel_multiplier=-1), reads=["onesb"], writes=["lstrict"])
        P.pool(lambda e: e.iota(eC, pattern=[[CAP, NE]], base=0, channel_multiplier=0, allow_small_or_imprecise_dtypes=True),
               writes=["eC"])
        P.pool(lambda e: e.iota(iota512, pattern=[[1, 512]], base=1, channel_multiplier=0, allow_small_or_imprecise_dtypes=True),
               writes=["iota512"])
        for pc in range(2):
            for hf in range(2):
                w = 2 ** (2 * pc + hf + 1)
                sl = slice(hf * 64, hf * 64 + 64)
                P.dve(lambda e, pc=pc, sl=sl, w=w: e.tensor_scalar(out=invc[sl, pc, :], in0=iota512[sl, 0:16], scalar1=float(w),
                                                                  scalar2=None, op0=ALU.min),
                      reads=["iota512"], writes=[("invc", pc, hf)])
                P.dve(lambda e, pc=pc, sl=sl: e.reciprocal(out=invc[sl, pc, :], in_=invc[sl, pc, :]),
                      reads=[("invc", pc, hf)], writes=[("invc", pc, hf)])
        P.pool(lambda e: e.memset(upT[:, :, 0:16], 0.0), writes=["upT_pad"])
        P.barrier(scr)

        def dma(out, in_, r=(), w=(), q="sp"):
            return P.dma(lambda e: e.dma_start(out=out, in_=in_), r, w, q)

        def dma_slow(out, in_, r=(), w=(), q="sp"):
            return P.dma(lambda e: e.dma_start(out=out, in_=in_, allow_slow_non_contiguous=True), r, w, q)

        def mm(out, lhsT, rhs, st, sp, r, w):
            return P.pe(lambda e: e.matmul(out, lhsT=lhsT, rhs=rhs, start=st, stop=sp), r, w)

        def tr(out, in_, ident, r, w):
            return P.pe(lambda e: e.transpose(out=out, in_=in_, identity=ident), r, w)

        def cp(eng, out, in_, r, w):
            if eng == "act":
                return P.act(lambda e: e.copy(out=out, in_=in_), r, w)
            return P.op(eng, lambda e: e.tensor_copy(out=out, in_=in_), r, w)

        def actf(out, in_, func, r, w, bias=None, scale=None):
            kw = {}
            if bias is not None:
                kw["bias"] = bias
            if scale is not None:
                kw["scale"] = scale
            return P.act(lambda e: e.activation(out=out, in_=in_, func=func, **kw), r, w)

        def ts(eng, out, in0, s1, s2, op0, op1, r, w):
            if op1 is None:
                return P.op(eng, lambda e: e.tensor_scalar(out=out, in0=in0, scalar1=s1, scalar2=None, op0=op0), r, w)
            return P.op(eng, lambda e: e.tensor_scalar(out=out, in0=in0, scalar1=s1, scalar2=s2, op0=op0, op1=op1), r, w)

        def tt(eng, out, in0, in1, op, r, w):
            return P.op(eng, lambda e: e.tensor_tensor(out=out, in0=in0, in1=in1, op=op), r, w)

        def stt(eng, out, in0, scalar, in1, op0, op1, r, w, accum=None):
            if accum is None:
                return P.op(eng, lambda e: e.scalar_tensor_tensor(out=out, in0=in0, scalar=scalar, in1=in1, op0=op0, op1=op1), r, w)
            return P.op(eng, lambda e: e.scalar_tensor_tensor(out=out, in0=in0, scalar=scalar, in1=in1, op0=op0, op1=op1,
                                                              accum_out=accum), r, w)

        def memset(eng, out, val, w):
            return P.op(eng, lambda e: e.memset(out, val), (), w)

        fin_ops = []
        rr = [0]
        regc = {}

        def negreg(e):
            if "neg" not in regc:
                regc["neg"] = e.to_reg(NEG)
            return regc["neg"]

        def nxt(n):
            rr[0] += 1
            return rr[0] % n

        for l in range(n_layers):
            xsrc = x_in if l == 0 else xs
            xdst = out if l == n_layers - 1 else xs

            A.reset(PERSIST)
            NA = 2056
            wA = A.alloc([128, 8, NA], BF16)
            xb = [A.alloc([128, D], BF16) for _ in range(3)]
            xTb = [A.alloc([128, 8, 512], BF16) for _ in range(2)]
            qst = [A.alloc([128, 4, 512], BF16) for _ in range(2)]
            vst = [A.alloc([128, 512], BF16) for _ in range(2)]
            memset("pool", upT[:, :, 0:16], 0.0, ["upT_pad"])
            groups = [(0, 512), (512, 512), (1024, 512), (1536, 520)]
            for gi, (c0, n) in enumerate(groups):
                dma(wA[:, :, c0:c0 + n], w_in[l][:, c0:c0 + n].rearrange("(kc p) n -> p kc n", p=128), w=[("wA", gi, 0), ("wA", gi, 1)], q="pool")
            wA_tags = [("wA", gi, h) for gi in range(4) for h in range(2)]
            psr = 0
            for tb in range(NB):
                s2 = tb % 2
                tbs = slice(tb * 512, (tb + 1) * 512)
                for ti in range(4):
                    i = tb * 4 + ti
                    s = i % 3
                    dma(xb[s], xsrc[i * 128:(i + 1) * 128, :], w=[("xb", s)], q="pool")
                    pk = 6 + (i % 2)
                    for kc in range(8):
                        tr(psb(pk)[:, kc, :], xb[s][:, kc * 128:(kc + 1) * 128], identb, [("xb", s), "identb"], [("ps", pk)])
                    cp("act" if i % 2 == 0 else "dve", xTb[s2][:, :, ti * 128:(ti + 1) * 128], psb(pk), [("ps", pk)], [("xTb", s2)])
                dma(xT_d[:, :, tbs], xTb[s2], [("xTb", s2)], ["xT_d"])
                rdA = wA_tags + [("xTb", s2)]
                for qi, (dst, scale) in enumerate(((qT_d, 0.125), (kT_d, 1.0))):
                    for sc in range(4):
                        pk = psr % 6
                        psr += 1
                        c = qi * 512 + sc * 128
                        for kc in range(8):
                            mm(PS[pk][:, :], wA[:, kc, c:c + 128], xTb[s2][:, kc, :], kc == 0, kc == 7, rdA, [("ps", pk)])
                        if sc % 2 == 0:
                            actf(qst[qi][:, sc, :], PS[pk][:, :], AF.Copy, [("ps", pk)], [("qst", qi)], scale=scale)
                        else:
                            ts("dve", qst[qi][:, sc, :], PS[pk][:, :], scale, None, ALU.mult, None, [("ps", pk)], [("qst", qi)])
                    dma(dst.rearrange("(sc p) t -> p sc t", p=128)[:, :, tbs], qst[qi], [("qst", qi)], [("qk_d", qi)])
                for ti in range(4):
                    pk = psr % 6
                    psr += 1
                    for kc in range(8):
                        mm(PS[pk][:, :], xTb[s2][:, kc, ti * 128:(ti + 1) * 128], wA[:, kc, 1024:1536], kc == 0, kc == 7, rdA, [("ps", pk)])
                    cp("act" if ti % 2 == 0 else "dve", vst[ti % 2], PS[pk][:, :], [("ps", pk)], [("vst", ti % 2)])
                    dma(v_d[(tb * 4 + ti) * 128:(tb * 4 + ti + 1) * 128, :], vst[ti % 2], [("vst", ti % 2)], ["v_d"])
                pk = psr % 6
                psr += 1
                for kc in range(8):
                    mm(PS[pk][0:8, :], wA[:, kc, 1536:1544], xTb[s2][:, kc, :], kc == 0, kc == 7, rdA, [("ps", pk)])
                cp("act", fT[:, tbs], PS[pk][0:8, :], [("ps", pk)], [("fT", tb)])
                for pc in range(2):
                    pk = psr % 6
                    psr += 1
                    c = 1544 + pc * 128
                    for kc in range(8):
                        mm(PS[pk][:, :], wA[:, kc, c:c + 128], xTb[s2][:, kc, :], kc == 0, kc == 7, rdA, [("ps", pk)])
                    cp("act", upT[:, pc, 16 + tb * 512:16 + (tb + 1) * 512], PS[pk][:, :], [("ps", pk)], [("upT", pc)])
                for sc in range(2):
                    pk = psr % 6
                    psr += 1
                    c = 1800 + sc * 128
                    for kc in range(8):
                        mm(PS[pk][:, :], wA[:, kc, c:c + 128], xTb[s2][:, kc, :], kc == 0, kc == 7, rdA, [("ps", pk)])
                    cp("dve", usT[:, sc, tbs], PS[pk][:, :], [("ps", pk)], [("usT", sc)])
            P.barrier(scr)
            if stop_after == "A":
                break

            A.reset(PERSIST)
            bfc = A.alloc([8, 1], F32)
            nbf = A.alloc([8, 1], F32)
            ef = fT
            cs = A.alloc([8, T], F32)
            ncs = fT
            csK = A.alloc([128, NT, 8], F32)
            dma(bfc, b_forget[l].rearrange("(h o) -> h o", o=1), w=["bfc"])
            ts("dve", nbf, bfc, -1.0, None, ALU.mult, None, ["bfc"], ["nbf"])
            fr = [("fT", tb) for tb in range(NB)]
            actf(ef, fT, AF.Exp, fr + ["nbf"], fr + ["ef"], bias=nbf, scale=-1.0)
            actf(ef, ef, AF.Ln, ["ef"], ["ef"], bias=1.0, scale=1.0)
            for hh in range(2):
                hs = slice(hh * 2048, (hh + 1) * 2048)
                init = 0.0 if hh == 0 else cs[:, 2047:2048]
                P.dve(lambda e, hs=hs, init=init: e.tensor_tensor_scan(out=cs[:, hs], data0=onesf[0:8, 0:1].to_broadcast([8, 2048]),
                                                                         data1=ef[:, hs], initial=init, op0=ALU.mult, op1=ALU.add),
                      ["ef", "onesf", "cs"], ["cs"])
            ts("dve", ncs, cs, -1.0, None, ALU.mult, None, ["cs", "ef"], ["ncs", "ef"] + fr)
            dma(c_d, ncs, ["ncs"], ["c_d"])
            for i in range(NT):
                tr(PS[5][:, i * 8:(i + 1) * 8], cs[:, i * 128:(i + 1) * 128], identf[0:8, 0:8], ["cs", "identf"], [("ps", 5)])
            cp("dve", csK, PS[5][:, 0:NT * 8].rearrange("p (i h) -> p i h", h=8), [("ps", 5)], ["csK"])

            qh = [A.alloc([64, T], BF16) for _ in range(2)]
            kh = [A.alloc([64, T], BF16) for _ in range(2)]
            V1 = [A.alloc([128, NT, 128], BF16) for _ in range(2)]
            cB0 = A.alloc([128, T], F32)
            cB = [cB0, cB0]
            NBUF = 4
            tmpb = [A.alloc([128, 512], F32) for _ in range(NBUF)]
            Ptb = [A.alloc([128, 512], BF16) for _ in range(NBUF)]
            rden = [A.alloc([64, 512], F32) for _ in range(2)]
            yst = [A.alloc([64, 512], BF16) for _ in range(2)]
            for s in range(2):
                memset("pool", V1[s][:, :, 64:128], 1.0, [("V1o", s)])
            steps = [(h, qb, j) for h in range(8) for qb in range(NB) for j in range(4 * qb + 4)]
            LA = 3

            def emit_front(t):
                h, qb, j = steps[t]
                s = h % 2
                if qb == 0 and j == 0:
                    dma(qh[s], qT_d[h * 64:(h + 1) * 64, :], [("qk_d", 0)], [("qh", s)])
                    dma(kh[s], kT_d[h * 64:(h + 1) * 64, :], [("qk_d", 1)], [("kh", s)])
                    dma(V1[s][:, :, 0:64], v_d.rearrange("(i p) c -> p i c", p=128)[:, :, h * 64:(h + 1) * 64], ["v_d"], [("V1", s)])
                    dma(cB[s], c_d[h:h + 1, :].partition_broadcast(128), ["c_d"], [("cB", 0)])
                qs = slice(qb * 512, (qb + 1) * 512)
                pk = t % 4
                sb = t % NBUF
                mm(PS[pk][:, :], kh[s][:, j * 128:(j + 1) * 128], qh[s][:, qs], True, True, [("qh", s), ("kh", s)], [("ps", pk)])
                tt("dve", tmpb[sb], PS[pk][:, :], cB[s][:, qs], ALU.add, [("ps", pk), ("cB", 0)], [("tmp", sb)])
                if j >= 4 * qb:
                    P.pool(lambda e, sb=sb, base=qb * 512 - j * 128: e.affine_select(
                        out=tmpb[sb], in_=tmpb[sb], pattern=[[1, 512]], compare_op=ALU.is_ge, fill=negreg(e),
                        base=base, channel_multiplier=-1), [("tmp", sb)], [("tmp", sb)])
                actf(Ptb[sb], tmpb[sb], AF.Exp, [("tmp", sb), "csK"], [("Pt", sb)], bias=csK[:, j, h:h + 1], scale=1.0)

            def emit_back(t):
                h, qb, j = steps[t]
                s = h % 2
                qs = slice(qb * 512, (qb + 1) * 512)
                sb = t % NBUF
                po = 4 + (qb % 2)
                nj = 4 * qb + 4
                mm(PS[po][:, :], V1[s][:, j, :], Ptb[sb], j == 0, j == nj - 1, [("V1", s), ("V1o", s), ("Pt", sb)], [("ps", po)])
                if j == nj - 1:
                    r2 = qb % 2
                    P.dve(lambda e, r2=r2, po=po: e.reciprocal(out=rden[r2], in_=PS[po][64:128, :]), [("ps", po)], [("rden", r2)])
                    tt("dve", yst[r2], PS[po][0:64, :], rden[r2], ALU.mult, [("ps", po), ("rden", r2)], [("yst", r2)])
                    dma(ya_d[(h % 2) * 64:(h % 2) * 64 + 64, h // 2, qs], yst[r2], [("yst", r2)], ["ya_d"])

            for t in range(len(steps) + LA):
                if t < len(steps):
                    emit_front(t)
                if t >= LA:
                    emit_back(t - LA)
            P.barrier(scr)
            if stop_after == "B2":
                break
            A.reset(PERSIST)
            wpf = A.alloc([128, 2, 128], F32)
            wpb = A.alloc([128, 2, 128], BF16)
            psc = A.alloc([128, 2], F32)
            sbuf4 = [A.alloc([128, 16 + T], F32) for _ in range(4)]
            mixT = A.alloc([128, 2, T], BF16)
            tmp16 = A.alloc([128, 2, 16], F32)
            ybst = [A.alloc([128, 512], BF16) for _ in range(2)]
            memset("pool", wpf, 0.0, ["wpf"])
            for g in range(4):
                hs = slice((g % 2) * 64, (g % 2) * 64 + 64)
                dma(wpf[hs, g // 2, (g % 2) * 64:(g % 2) * 64 + 64], w_pool[l, g], w=["wpf"])
            cp("dve", wpb, wpf, ["wpf"], ["wpb"])
            dma_slow(psc, pool_scale[l].rearrange("(c p) -> p c", p=128), w=["psc"])
            for pc in range(2):
                eng = "dve" if pc == 0 else "pool"
                a_, b_ = sbuf4[2 * pc], sbuf4[2 * pc + 1]
                ta, tb_ = ("sA", pc), ("sB", pc)
                memset(eng, a_[:, 0:16], 0.0, [ta])
                memset(eng, b_[:, 0:16], 0.0, [tb_])
                u = upT[:, pc, :]
                ut = ("upT", pc)
                tt(eng, a_[:, 16:], u[:, 16:], u[:, 15:15 + T], ALU.add, [ut, "upT_pad"], [ta])
                tt(eng, b_[:, 16:], a_[:, 16:], a_[:, 14:14 + T], ALU.add, [ta], [tb_])
                if pc == 1:
                    tt(eng, a_[:, 16:], b_[:, 16:], b_[:, 12:12 + T], ALU.add, [tb_], [ta])
                    tt(eng, b_[:, 16:], a_[:, 16:], a_[:, 8:8 + T], ALU.add, [ta], [tb_])
                for hf in range(2):
                    sl = slice(hf * 64, hf * 64 + 64)
                    src, st_ = (a_, ta) if hf == 0 else (b_, tb_)
                    w = 2 ** (2 * pc + hf + 1)
                    stt("dve", mixT[sl, pc, :], src[sl, 16:], 1.0 / w, u[sl, 16:], ALU.mult, ALU.subtract, [st_, ut], [("mixT", pc, hf)])
                    tt(eng, tmp16[sl, pc, :], src[sl, 16:32], invc[sl, pc, :], ALU.mult, [st_, ("invc", pc, hf)], [("tmp16", pc, hf)])
                    tt(eng, mixT[sl, pc, 0:16], tmp16[sl, pc, :], u[sl, 16:32], ALU.subtract, [("tmp16", pc, hf), ut], [("mixT", pc, hf)])
            for pc in range(2):
                for tb in range(NB):
                    tbs = slice(tb * 512, (tb + 1) * 512)
                    pk = nxt(4)
                    s = nxt(2)
                    mm(PS[pk][:, :], wpb[:, pc, :], mixT[:, pc, tbs], True, True, ["wpb", ("mixT", pc, 0), ("mixT", pc, 1)], [("ps", pk)])
                    actf(ybst[s], PS[pk][:, :], AF.Copy, [("ps", pk), "psc"], [("ybst", s)], scale=psc[:, pc:pc + 1])
                    dma(yb_d[:, pc, tbs], ybst[s], [("ybst", s)], ["yb_d"])
            P.barrier(scr)
            if stop_after == "B3":
                break

            A.reset(PERSIST)
            TWO_PI = 6.28318
            p8 = {n: A.alloc([128, 8], F32) for n in ("lr", "li", "ldt", "dt", "tmp", "mag", "th", "v8", "sth", "cth", "ar", "ai",
                                                      "den", "nr", "zr", "zi", "t1", "t2")}
            pi8 = A.alloc([128, 8], I32)
            scr512 = [A.alloc([128, 512], F32) for _ in range(3)]
            scri = A.alloc([128, 512], I32)
            cosT = A.alloc([128, 8, 512], BF16)
            sinT = A.alloc([128, 8, 512], BF16)
            rhoT = A.alloc([128, 8, 512], F32)
            br_ = A.alloc([128, 8, 16], F32)
            bi_ = A.alloc([128, 8, 16], F32)
            bt = [A.alloc([128, 8, 16], F32) for _ in range(4)]
            Bb = [A.alloc([128, 8, 16], F32) for _ in range(2)]
            Bsrc = A.alloc([128, 2, 8, 128], F32)
            Csrc = A.alloc([128, 2, 8, 128], F32)
            LB = A.alloc([128, 2, 8, 128], BF16)
            LC = A.alloc([128, 2, 8, 128], BF16)
            Dcol = A.alloc([128, 2], F32)
            bglu = A.alloc([128, 2], F32)
            wgs = A.alloc([128, 2, 256], F32)
            wgl = A.alloc([128, 2, 256], BF16)
            hrp = A.alloc([128, 8], F32)
            hip = A.alloc([128, 8], F32)
            NWK = 14
            wk = [[A.alloc([128, 512], BF16) for _ in range(NWK)] for _ in range(2)]
            ypre = [A.alloc([128, 512], F32) for _ in range(2)]
            ygT = A.alloc([128, 2, 512], BF16)
            sgb = [A.alloc([128, 512], F32) for _ in range(2)]
            ycst = [A.alloc([128, 512], BF16) for _ in range(2)]

            def sincos(out_s, out_c, vt, rtag, wtag_s, wtag_c, shape_i, shape_f):
                fi, f1, f2 = shape_i, shape_f[0], shape_f[1]
                for which, outp, wtag in ((0, out_s, wtag_s), (1, out_c, wtag_c)):
                    if which == 1:
                        ts("dve", f2, vt, 0.25, None, ALU.add, None, [rtag], [("sc_f2",)])
                        src, srct = f2, ("sc_f2",)
                    else:
                        src, srct = vt, rtag
                    cp("dve", fi, src, [srct], [("sc_i",)])
                    cp("dve", f1, fi, [("sc_i",)], [("sc_f1",)])
                    tt("dve", f1, src, f1, ALU.subtract, [srct, ("sc_f1",)], [("sc_f1",)])
                    actf(outp, f1, AF.Sin, [("sc_f1",)], [wtag], scale=TWO_PI)

            for gg in range(2):
                hs = slice(gg * 64, gg * 64 + 64)
                dma_slow(p8["lr"][hs, :], lam_re[l].rearrange("(i gg) p -> gg p i", gg=2)[gg], w=["lr"])
                dma_slow(p8["li"][hs, :], lam_im[l].rearrange("(i gg) p -> gg p i", gg=2)[gg], w=["li"])
                dma_slow(p8["ldt"][hs, :], log_dt[l][gg::2].partition_broadcast(64), w=["ldt"])
                dma(br_[hs], b_re[l].rearrange("(i gg) p h -> gg p i h", gg=2)[gg], w=["br"])
                dma(bi_[hs], b_im[l].rearrange("(i gg) p h -> gg p i h", gg=2)[gg], w=["bi"])
            actf(p8["dt"], p8["ldt"], AF.Exp, ["ldt"], ["dt"])
            tt("dve", p8["tmp"], p8["lr"], p8["dt"], ALU.mult, ["lr", "dt"], ["tmp8"])
            actf(p8["mag"], p8["tmp"], AF.Exp, ["tmp8"], ["mag"])
            tt("dve", p8["th"], p8["li"], p8["dt"], ALU.mult, ["li", "dt"], ["th"])
            ts("dve", p8["v8"], p8["th"], 1.0 / (2.0 * math.pi), None, ALU.mult, None, ["th"], ["v8"])
            sincos(p8["sth"], p8["cth"], p8["v8"], "v8", "sth", "cth", pi8, (p8["t1"], p8["t2"]))
            tt("dve", p8["ar"], p8["mag"], p8["cth"], ALU.mult, ["mag", "cth"], ["ar"])
            tt("dve", p8["ai"], p8["mag"], p8["sth"], ALU.mult, ["mag", "sth"], ["ai"])
            tt("dve", p8["den"], p8["lr"], p8["lr"], ALU.mult, ["lr"], ["den"])
            tt("dve", p8["t1"], p8["li"], p8["li"], ALU.mult, ["li", ("sc_f1",)], ["t1"])
            tt("dve", p8["den"], p8["den"], p8["t1"], ALU.add, ["den", "t1"], ["den"])
            P.dve(lambda e: e.reciprocal(out=p8["den"], in_=p8["den"]), ["den"], ["den"])
            ts("dve", p8["nr"], p8["ar"], -1.0, None, ALU.add, None, ["ar"], ["nr"])
            tt("dve", p8["t1"], p8["nr"], p8["lr"], ALU.mult, ["nr", "lr", "t1"], ["t1"])
            tt("dve", p8["t2"], p8["ai"], p8["li"], ALU.mult, ["ai", "li", ("sc_f2",)], ["t2"])
            tt("dve", p8["zr"], p8["t1"], p8["t2"], ALU.add, ["t1", "t2"], ["zr"])
            tt("dve", p8["zr"], p8["zr"], p8["den"], ALU.mult, ["zr", "den"], ["zr"])
            tt("dve", p8["t1"], p8["ai"], p8["lr"], ALU.mult, ["ai", "lr", "t1", "zr"], ["t1"])
            tt("dve", p8["t2"], p8["nr"], p8["li"], ALU.mult, ["nr", "li", "t2", "zr"], ["t2"])
            tt("dve", p8["zi"], p8["t1"], p8["t2"], ALU.subtract, ["t1", "t2"], ["zi"])
            tt("dve", p8["zi"], p8["zi"], p8["den"], ALU.mult, ["zi", "den"], ["zi"])
            for i in range(8):
                ts("dve", scr512[0], iota512, p8["v8"][:, i:i + 1], None, ALU.mult, None, ["iota512", "v8", ("tab", i - 1)], [("vt",)])
                sincos(sinT[:, i, :], cosT[:, i, :], scr512[0], ("vt",), ("sinT", i), ("cosT", i), scri, (scr512[1], scr512[2]))
                ts("pool", rhoT[:, i, :], onesf, p8["mag"][:, i:i + 1], None, ALU.mult, None, ["onesf", "mag", ("sinT", i), ("cosT", i)], [("rho", i), ("tab", i)])
            tabr = [("sinT", i) for i in range(8)] + [("cosT", i) for i in range(8)]
            zrb = p8["zr"].unsqueeze(2).to_broadcast([128, 8, 16])
            zib = p8["zi"].unsqueeze(2).to_broadcast([128, 8, 16])
            tt("dve", bt[0], br_, zrb, ALU.mult, ["br", "zr"], ["bt0"])
            tt("dve", bt[1], bi_, zib, ALU.mult, ["bi", "zi"], ["bt1"])
            tt("dve", Bb[0], bt[0], bt[1], ALU.subtract, ["bt0", "bt1"], ["Bb0"])
            tt("dve", bt[2], bi_, zrb, ALU.mult, ["bi", "zr"], ["bt2"])
            tt("dve", bt[3], br_, zib, ALU.mult, ["br", "zi"], ["bt3"])
            tt("dve", Bb[1], bt[2], bt[3], ALU.add, ["bt2", "bt3"], ["Bb1"])
            memset("pool", Bsrc, 0.0, ["Bsrc"])
            memset("pool", Csrc, 0.0, ["Csrc"])
            k_ = 0
            for ri in range(2):
                for i in range(8):
                    for gg in range(2):
                        hs = slice(gg * 64, gg * 64 + 64)
                        c0 = 32 * (i % 4) + 16 * gg
                        cp(("dve", "pool", "act")[k_ % 3], Bsrc[hs, ri, i, c0:c0 + 16], Bb[ri][hs, i, :], [f"Bb{ri}", "Bsrc"], ["Bsrc"])
                        k_ += 1
            for ri in range(2):
                for i0 in (0, 4):
                    pk = nxt(4)
                    for i in range(i0, i0 + 4):
                        tr(PS[pk][:, (i % 4) * 128:(i % 4) * 128 + 128], Bsrc[:, ri, i, :], identf, ["Bsrc", "identf"], [("ps", pk)])
                    cp("act", LB[:, ri, i0:i0 + 4, :], PS[pk][:, :].rearrange("p (a b) -> p a b", a=4), [("ps", pk)], ["LB"])
            csrc_ap = (c_re, c_im)
            for ri in range(2):
                for gg in range(2):
                    hs = slice(gg * 64, gg * 64 + 64)
                    for m in range(4):
                        c0 = 32 * m + 16 * gg
                        for j in range(2):
                            dma_slow(Csrc[hs, ri, m + 4 * j, c0:c0 + 16], csrc_ap[ri][l, 2 * m + gg + 8 * j].rearrange("h p -> p h"),
                                     r=["Csrc"], w=["Csrc"])
            cp("dve", LC[:, 0], Csrc[:, 0], ["Csrc"], ["LC"])
            ts("dve", LC[:, 1], Csrc[:, 1], -1.0, None, ALU.mult, None, ["Csrc"], ["LC"])
            dma_slow(Dcol, ssm_d[l].rearrange("g h -> (g h)").rearrange("(c p) -> p c", p=128), w=["Dcol"])
            dma_slow(bglu, b_glu[l].rearrange("(c p) -> p c", p=128), w=["bglu"])
            dma(wgs, w_glu[l].rearrange("(kc p) n -> p kc n", p=128), w=["wgs"])
            cp("pool", wgl, wgs, ["wgs"], ["wgl"])
            memset("pool", hrp, 0.0, [("hrp", i) for i in range(8)])
            memset("pool", hip, 0.0, [("hip", i) for i in range(8)])
            units = [(tb, i) for tb in range(NB) for i in range(8)]

            def s5_front(u):
                tb, i = units[u]
                tbs = slice(tb * 512, (tb + 1) * 512)
                ch = i // 4
                s = u % 2
                W = wk[s]
                wt = lambda n: ("wk", s, n)
                pxr = 2 * s
                pxi = 2 * s + 1
                us = usT[:, ch, tbs]
                mm(PS[pxr][:, :], LB[:, 0, i, :], us, True, True, ["LB", ("usT", ch)], [("ps", pxr)])
                mm(PS[pxi][:, :], LB[:, 1, i, :], us, True, True, ["LB", ("usT", ch)], [("ps", pxi)])
                cT_, sT_ = cosT[:, i, :], sinT[:, i, :]
                tt("dve", W[0], PS[pxr][:, :], cT_, ALU.mult, [("ps", pxr)] + tabr, [wt(0)])
                tt("dve", W[1], PS[pxi][:, :], sT_, ALU.mult, [("ps", pxi)] + tabr, [wt(1)])
                tt("pool", W[4], W[0], W[1], ALU.add, [wt(0), wt(1)], [wt(4)])
                tt("dve", W[2], PS[pxi][:, :], cT_, ALU.mult, [("ps", pxi)] + tabr, [wt(2)])
                tt("dve", W[3], PS[pxr][:, :], sT_, ALU.mult, [("ps", pxr)] + tabr, [wt(3)])
                tt("pool", W[5], W[2], W[3], ALU.subtract, [wt(2), wt(3)], [wt(5)])
                P.dve(lambda e, W=W, i=i: e.tensor_tensor_scan(out=W[6], data0=rhoT[:, i, :], data1=W[4], initial=hrp[:, i:i + 1],
                                                               op0=ALU.mult, op1=ALU.add), [wt(4), ("rho", i), ("hrp", i)], [wt(6)])
                P.dve(lambda e, W=W, i=i: e.tensor_tensor_scan(out=W[7], data0=rhoT[:, i, :], data1=W[5], initial=hip[:, i:i + 1],
                                                               op0=ALU.mult, op1=ALU.add), [wt(5), ("rho", i), ("hip", i)], [wt(7)])
                tt("pool", W[8], W[6], cT_, ALU.mult, [wt(6)] + tabr, [wt(8)])
                tt("pool", W[9], W[7], sT_, ALU.mult, [wt(7)] + tabr, [wt(9)])
                tt("pool", W[12], W[8], W[9], ALU.subtract, [wt(8), wt(9)], [wt(12)])
                tt("dve", W[10], W[7], cT_, ALU.mult, [wt(7)] + tabr, [wt(10)])
                tt("dve", W[11], W[6], sT_, ALU.mult, [wt(6)] + tabr, [wt(11)])
                tt("pool", W[13], W[10], W[11], ALU.add, [wt(10), wt(11)], [wt(13)])
                cp("act", hrp[:, i:i + 1], W[12][:, 511:512], [wt(12)], [("hrp", i)])
                cp("act", hip[:, i:i + 1], W[13][:, 511:512], [wt(13)], [("hip", i)])

            def s5_back(u):
                tb, i = units[u]
                tbs = slice(tb * 512, (tb + 1) * 512)
                ch = i // 4
                s = u % 2
                W = wk[s]
                wt = lambda n: ("wk", s, n)
                us = usT[:, ch, tbs]
                py = 4 + ch
                mm(PS[py][:, :], LC[:, 0, i, :], W[12], i % 4 == 0, False, ["LC", wt(12)], [("ps", py)])
                mm(PS[py][:, :], LC[:, 1, i, :], W[13], False, i % 4 == 3, ["LC", wt(13)], [("ps", py)])
                if i % 4 == 3:
                    stt("dve", ypre[ch], us, Dcol[:, ch:ch + 1], PS[py][:, :], ALU.mult, ALU.add, [("usT", ch), "Dcol", ("ps", py)], [("ypre", ch)])
                    actf(ygT[:, ch, :], ypre[ch], AF.Gelu_apprx_tanh, [("ypre", ch)], [("ygT", ch)])
                if i == 7:
                    for oc in range(2):
                        pz = 6 + oc
                        for kc in range(2):
                            mm(PS[pz][:, :], wgl[:, kc, oc * 128:(oc + 1) * 128], ygT[:, kc, :], kc == 0, kc == 1,
                               ["wgl", ("ygT", 0), ("ygT", 1)], [("ps", pz)])
                        actf(sgb[oc], PS[pz][:, :], AF.Sigmoid, [("ps", pz), "bglu"], [("sgb", oc)], bias=bglu[:, oc:oc + 1], scale=1.0)
                        tt("pool", ycst[oc], ygT[:, oc, :], sgb[oc], ALU.mult, [("ygT", oc), ("sgb", oc)], [("ycst", oc)])
                        dma(yc_d[:, oc, tbs], ycst[oc], [("ycst", oc)], ["yc_d"])

            for u in range(len(units) + 1):
                if u < len(units):
                    s5_front(u)
                if u >= 1:
                    s5_back(u - 1)
            P.barrier(scr)
            if stop_after == "B4":
                break
            A.reset(PERSIST_C)
            wg = A.alloc([128, 8, 3072], BF16)
            wbr = A.alloc([128, 8, D], BF16)
            wo = A.alloc([128, 8, D], BF16)
            wrs = A.alloc([128, 8, NE], F32)
            wrb = A.alloc([128, 8, NE], BF16)
            gB = A.alloc([128, D], F32)
            bB = A.alloc([128, D], F32)
            brB = A.alloc([128, NE], F32)
            xTc = [A.alloc([128, 8, 512], BF16) for _ in range(2)]
            ybk = [A.alloc([128, 8, 512], BF16) for _ in range(2)]
            mrg = A.alloc([128, 8, 512], BF16)
            sgt = [A.alloc([128, 512], F32) for _ in range(2)]
            macc = [A.alloc([128, 512], F32) for _ in range(2)]
            mtmp = [A.alloc([128, 512], F32) for _ in range(2)]
            xw = [A.alloc([128, D], F32) for _ in range(2)]
            x1b = [A.alloc([128, D], BF16) for _ in range(2)]
            x1T = A.alloc([128, 8, 128], BF16)
            sm = {n: A.alloc([128, NE], F32) for n in ("lg", "maskf", "ex", "exm", "valid", "d0", "d1", "Wd", "junk", "junk2")}
            mx8 = A.alloc([128, 8], F32)
            sm1 = {n: A.alloc([128, 1], F32) for n in ("nmx", "ssum", "rsum", "rs", "nmr")}
            dsel = A.alloc([128, 4], F32)
            bst = A.alloc([128, 2, 6], F32)
            mv = A.alloc([128, 2], F32)

            def wload(dst_ap, src_ap, wtag):
                dma(dst_ap, src_ap.rearrange("(kc p) n -> p kc n", p=128), w=[wtag], q="pool")

            for b in range(3):
                for half in range(2):
                    c0 = 2056 + b * 1024 + half * 512
                    wload(wg[:, :, b * 1024 + half * 512:b * 1024 + half * 512 + 512], w_in[l][:, c0:c0 + 512], "wg")
            for half in range(2):
                hs = slice(half * 512, half * 512 + 512)
                wload(wbr[:, 0:4, hs], w_br[0][l][:, hs], "wbr")
                wload(wbr[:, 4:6, hs], w_br[1][l][:, hs], "wbr")
                wload(wbr[:, 6:8, hs], w_br[2][l][:, hs], "wbr")
                wload(wo[:, :, hs], w_out[l][:, hs], "wo")
            dma(wrs, w_router[l].rearrange("(kc p) n -> p kc n", p=128), w=["wrs"])
            cp("dve", wrb, wrs, ["wrs"], ["wrb"])

            def load_ln(which, gB_, bB_):
                dma(gB_, ln_g[which][l].partition_broadcast(128), w=["gB"])
                dma(bB_, ln_b[which][l].partition_broadcast(128), w=["bB"])

            def layernorm(xw_ap, xtag, gB_, bB_, bst_, mv_, rs_, nmr_):
                for half in range(2):
                    P.dve(lambda e, half=half: e.bn_stats(out=bst_[:, half, :], in_=xw_ap[:, half * 512:(half + 1) * 512]), [xtag], ["bst"])
                P.dve(lambda e: e.bn_aggr(out=mv_, in_=bst_[:, :, :].rearrange("p a b -> p (a b)")), ["bst"], ["mv"])
                actf(rs_, mv_[:, 1:2], AF.Sqrt, ["mv", "eps_c"], ["rs"], bias=eps_c[:, 0:1], scale=1.0)
                P.dve(lambda e: e.reciprocal(out=rs_, in_=rs_), ["rs"], ["rs"])
                stt("dve", nmr_, mv_[:, 0:1], -1.0, rs_, ALU.mult, ALU.mult, ["mv", "rs"], ["nmr"])
                actf(xw_ap, xw_ap, AF.Identity, [xtag, "rs", "nmr"], [xtag], bias=nmr_[:, 0:1], scale=rs_[:, 0:1])
                tt("pool", xw_ap, xw_ap, gB_, ALU.mult, [xtag, "gB"], [xtag])
                tt("pool", xw_ap, xw_ap, bB_, ALU.add, [xtag, "bB"], [xtag])

            load_ln(0, gB, bB)
            dma(brB, b_router[l].partition_broadcast(128), w=["brB"])
            kq = 0
            for tb in range(NB):
                tbs = slice(tb * 512, (tb + 1) * 512)
                s = tb % 2
                dma(xTc[s], xT_d[:, :, tbs], ["xT_d"], [("xTc", s)])
                dma(ybk[s][:, 0:4, :], ya_d[:, :, tbs], ["ya_d"], [("ybk", s)])
                dma(ybk[s][:, 4:6, :], yb_d[:, :, tbs], ["yb_d"], [("ybk", s)])
                dma(ybk[s][:, 6:8, :], yc_d[:, :, tbs], ["yc_d"], [("ybk", s)])
                for c in range(8):
                    cs_ = slice(c * 128, (c + 1) * 128)
                    ma = macc[c % 2]
                    for b in range(3):
                        q2 = kq % 2
                        kq += 1
                        pg, pm = 2 * q2, 2 * q2 + 1
                        for kc in range(8):
                            mm(PS[pg][:, :], wg[:, kc, b * 1024 + c * 128:b * 1024 + c * 128 + 128], xTc[s][:, kc, :], kc == 0, kc == 7,
                               ["wg", ("xTc", s)], [("ps", pg)])
                        kcs = ((0, 1, 2, 3), (4, 5), (6, 7))[b]
                        for kc in kcs:
                            mm(PS[pm][:, :], wbr[:, kc, cs_], ybk[s][:, kc, :], kc == kcs[0], kc == kcs[-1], ["wbr", ("ybk", s)], [("ps", pm)])
                        actf(sgt[q2], PS[pg][:, :], AF.Sigmoid, [("ps", pg)], [("sgt", q2)])
                        if b == 0:
                            tt("dve", ma, PS[pm][:, :], sgt[q2], ALU.mult, [("ps", pm), ("sgt", q2)], [("macc", c % 2)])
                        else:
                            tt("dve", mtmp[b - 1], PS[pm][:, :], sgt[q2], ALU.mult, [("ps", pm), ("sgt", q2)], [("mtmp", b - 1)])
                            if b == 1:
                                tt("pool", ma, ma, mtmp[0], ALU.add, [("macc", c % 2), ("mtmp", 0)], [("macc", c % 2)])
                            else:
                                tt("pool", mrg[:, c, :], ma, mtmp[1], ALU.add, [("macc", c % 2), ("mtmp", 1)], [("mrg", c)])
                mrg_r = [("mrg", c) for c in range(8)]
                for ti in range(4):
                    i = tb * 4 + ti
                    s1 = i % 2
                    xt_ = ("xw", s1)
                    dma(xw[s1], xsrc[i * 128:(i + 1) * 128, :], w=[xt_])
                    for half in range(2):
                        pk = 6 + half
                        for kc in range(8):
                            mm(PS[pk][:, :], mrg[:, kc, ti * 128:(ti + 1) * 128], wo[:, kc, half * 512:(half + 1) * 512], kc == 0, kc == 7,
                               mrg_r + ["wo"], [("ps", pk)])
                        hs = slice(half * 512, (half + 1) * 512)
                        stt("dve", xw[s1][:, hs], xw[s1][:, hs], ALPHA, PS[pk][:, :], ALU.mult, ALU.add, [xt_, ("ps", pk)], [xt_])
                    layernorm(xw[s1], xt_, gB, bB, bst, mv, sm1["rs"], sm1["nmr"])
                    dma(x1_d[i * 128:(i + 1) * 128, :], xw[s1], [xt_], ["x1_d"])
                    cp("act", x1b[s1], xw[s1], [xt_], [("x1b", s1)])
                    for kc in range(8):
                        tr(psb(4)[:, kc, :], x1b[s1][:, kc * 128:(kc + 1) * 128], identb, [("x1b", s1), "identb"], [("ps", 4)])
                    cp("act", x1T, psb(4), [("ps", 4)], ["x1T"])
                    for kc in range(8):
                        mm(PS[5][:, 0:NE], x1T[:, kc, :], wrb[:, kc, :], kc == 0, kc == 7, ["x1T", "wrb"], [("ps5", "lg")])
                    tt("dve", sm["lg"], PS[5][:, 0:NE], brB, ALU.add, [("ps5", "lg"), "brB"], ["lg"])
                    P.dve(lambda e: e.max(out=mx8, in_=sm["lg"]), ["lg"], ["mx8"])
                    ts("dve", sm["maskf"], sm["lg"], mx8[:, 3:4], None, ALU.is_ge, None, ["lg", "mx8"], ["maskf"])
                    cp("pool", masks[:, i, :], sm["maskf"], ["maskf"], [("masks", i)])
                    ts("dve", sm1["nmx"], mx8[:, 0:1], -1.0, None, ALU.mult, None, ["mx8"], ["nmx"])
                    actf(sm["ex"], sm["lg"], AF.Exp, ["lg", "nmx"], ["ex"], bias=sm1["nmx"][:, 0:1], scale=1.0)
                    stt("dve", sm["exm"], sm["ex"], 1.0, sm["maskf"], ALU.mult, ALU.mult, ["ex", "maskf"], ["exm", "ssum"], accum=sm1["ssum"][:, 0:1])
                    P.dve(lambda e: e.reciprocal(out=sm1["rsum"], in_=sm1["ssum"]), ["ssum"], ["rsum"])
                    mm(PS[5][:, 64:64 + NE], lstrict, masks[:, i, :], True, i == 0, ["lstrict", ("masks", i)], [("ps5", "pos")])
                    for j in range(i):
                        mm(PS[5][:, 64:64 + NE], onesb, masks[:, j, :], False, j == i - 1, ["onesb", ("masks", j)], [("ps5", "pos")])
                    ts("dve", sm["valid"], PS[5][:, 64:64 + NE], float(CAP), None, ALU.is_lt, None, [("ps5", "pos")], ["valid"])
                    tt("dve", sm["d0"], PS[5][:, 64:64 + NE], eC, ALU.add, [("ps5", "pos"), "eC"], ["d0"])
                    stt("dve", sm["d1"], sm["d0"], float(DUMMY), sm["valid"], ALU.subtract, ALU.mult, ["d0", "valid"], ["d1"])
                    stt("dve", sm["Wd"], sm["exm"], sm1["rsum"][:, 0:1], sm["valid"], ALU.mult, ALU.mult, ["exm", "rsum", "valid"], ["Wd"])
                    for k in range(4):
                        stt("dve", sm["junk"], sm["lg"], mx8[:, k:k + 1], sm["d1"], ALU.is_equal, ALU.mult, ["lg", "mx8", "d1"], ["junk", "dsel"],
                            accum=dsel[:, k:k + 1])
                        stt("dve", sm["junk2"], sm["lg"], mx8[:, k:k + 1], sm["Wd"], ALU.is_equal, ALU.mult, ["lg", "mx8", "Wd"],
                            ["junk2", ("wsels", i)], accum=wsels[:, i, k:k + 1])
                    ts("dve", idxs[:, i, :], dsel, float(DUMMY), None, ALU.add, None, ["dsel"], [("idxs", i)])
                    tr(PS[5][0:NE, 128:256], sm["Wd"], identf, ["Wd", "identf"], [("ps5", "wdt")])
                    cp("act", WdTs[:, i, :], PS[5][0:NE, 128:256], [("ps5", "wdt")], [("WdTs", i)])
                    for k in range(4):
                        P.dma(lambda e, i=i, k=k, s1=s1: e.indirect_dma_start(
                            out=xg_d, out_offset=bass.IndirectOffsetOnAxis(ap=idxs[:, i, k:k + 1], axis=0), in_=x1b[s1], in_offset=None),
                            [("x1b", s1), ("idxs", i)], ["xg_d"], q="pool")
            P.barrier(scr)
            if stop_after == "C":
                break

            A.reset(PERSIST_C)
            bupA = A.alloc([128, NE, 8, 2], F32)
            wu = [A.alloc([128, 8, 512], BF16) for _ in range(8)]
            wdn = [A.alloc([128, 8, 512], BF16) for _ in range(4)]
            xgl = A.alloc([128, CT, D], BF16)
            xgT = [A.alloc([128, 8, CAP], BF16) for _ in range(2)]
            actT = A.alloc([128, 8, CAP], BF16)
            yst2 = [A.alloc([128, 512], F32) for _ in range(6)]
            ET = {n: [A.alloc([128, 512], F32) for _ in range(3)] for n in ("g", "s", "l", "gs")}
            for e4 in range(0, NE, 4):
                dma_slow(bupA[:, e4:e4 + 4], b_up[l, e4:e4 + 4].rearrange("e (c p two) -> p e c two", p=128, two=2), w=["bupA"])
            ts("dve", bupA[:, :, :, 1:2], bupA[:, :, :, 1:2], 1.0, None, ALU.add, None, ["bupA"], ["bupA"])
            eD = [0]

            def load_up(ex, blk):
                sl = (ex % 2) * 4 + blk
                dma(wu[sl], w_up[l, ex][:, blk * 512:(blk + 1) * 512].rearrange("(kc p) n -> p kc n", p=128), w=[("wu", sl)], q="pool")

            def load_dn(ex, half):
                sl = (ex % 2) * 2 + half
                dma(wdn[sl], w_down[l, ex][:, half * 512:(half + 1) * 512].rearrange("(kc p) n -> p kc n", p=128), w=[("wdn", sl)], q="pool")

            def load_tok(ex):
                dma(xgl, xg_d[ex * CAP:(ex + 1) * CAP, :].rearrange("(i p) d -> p i d", p=128), ["xg_d"], ["xgl"])

            def transposes(ex):
                xs_ = ex % 2
                for it in range(CT):
                    pk = 6 + (it % 2)
                    for kc in range(8):
                        tr(psb(pk)[:, kc, :], xgl[:, it, kc * 128:(kc + 1) * 128], identb, ["xgl", "identb"], [("ps", pk)])
                    cp("act" if it % 2 == 0 else "dve", xgT[xs_][:, :, it * 128:(it + 1) * 128], psb(pk), [("ps", pk)], [("xgT", xs_)])

            def up_block(ex, blk):
                xs_ = ex % 2
                for fc in range(2):
                    ffc = blk * 2 + fc
                    for (n0, n) in ((0, 512), (512, CAP - 512)):
                        q2 = eD[0] % 3
                        eD[0] += 1
                        pg, pl = 2 * q2, 2 * q2 + 1
                        for gl, pk in ((0, pg), (1, pl)):
                            for kc in range(8):
                                mm(PS[pk][:, 0:n], wu[(ex % 2) * 4 + blk][:, kc, fc * 256 + gl:fc * 256 + 256:2], xgT[xs_][:, kc, n0:n0 + n], kc == 0, kc == 7,
                                   [("wu", (ex % 2) * 4 + blk), ("xgT", xs_)], [("ps", pk)])
                        g, sg_, l_, gs = (ET[nm][q2][:, 0:n] for nm in ("g", "s", "l", "gs"))
                        et = lambda nm: ("ET", nm, q2)
                        ts("dve", g, PS[pg][:, 0:n], bupA[:, ex, ffc, 0:1], 7.0, ALU.add, ALU.min, [("ps", pg), "bupA"], [et("g")])
                        actf(sg_, g, AF.Sigmoid, [et("g")], [et("s")], scale=1.702)
                        ts("dve", l_, PS[pl][:, 0:n], bupA[:, ex, ffc, 1:2], 8.0, ALU.add, ALU.min, [("ps", pl), "bupA"], [et("l")])
                        tt("pool", gs, g, sg_, ALU.mult, [et("g"), et("s")], [et("gs")])
                        stt("dve", actT[:, ffc, n0:n0 + n], l_, -6.0, gs, ALU.max, ALU.mult, [et("gs"), et("l")], [("actT", ffc)])

            def down_half(ex, half):
                actT_r = [("actT", f) for f in range(8)]
                for it in range(CT):
                    ys = it % 6
                    pk = 6 + (it % 2)
                    for kc in range(8):
                        mm(PS[pk][:, :], actT[:, kc, it * 128:(it + 1) * 128], wdn[(ex % 2) * 2 + half][:, kc, :], kc == 0, kc == 7,
                           actT_r + [("wdn", (ex % 2) * 2 + half)], [("ps", pk)])
                    cp("act", yst2[ys][:, 0:512], PS[pk][:, :], [("ps", pk)], [("yst2", ys)])
                    dma(yg_d[ex * CAP + it * 128:ex * CAP + (it + 1) * 128, half * 512:(half + 1) * 512], yst2[ys][:, 0:512],
                        [("yst2", ys)], ["yg_d"], q="act")

            for blk in range(4):
                load_up(0, blk)
            for half in range(2):
                load_dn(0, half)
            load_tok(0)
            transposes(0)
            for ex in range(NE):
                nx = ex + 1 < NE
                if nx:
                    load_tok(ex + 1)
                for blk in range(4):
                    if nx:
                        load_up(ex + 1, blk)
                    up_block(ex, blk)
                if nx:
                    transposes(ex + 1)
                for half in range(2):
                    if nx:
                        load_dn(ex + 1, half)
                    down_half(ex, half)
            P.barrier(scr)
            if stop_after == "D":
                break

            A.reset(PERSIST_C)
            gB2 = A.alloc([128, D], F32)
            bB2 = A.alloc([128, D], F32)
            bdn = A.alloc([32, D], F32)
            xw2 = [A.alloc([128, D], F32) for _ in range(2)]
            yk = [[A.alloc([128, D], F32) for _ in range(4)] for _ in range(2)]
            accB = [A.alloc([128, D], F32) for _ in range(2)]
            sm1e = {n: A.alloc([128, 1], F32) for n in ("rs", "nmr")}
            bste = A.alloc([128, 2, 6], F32)
            mve = A.alloc([128, 2], F32)
            load_ln(1, gB2, bB2)
            dma(bdn, b_down[l], w=["bdn"])
            def e_front(i):
                s1 = i % 2
                dma(xw2[s1], x1_d[i * 128:(i + 1) * 128, :], ["x1_d"], [("xw", s1)])
                for k in range(4):
                    P.dma(lambda e, i=i, k=k, s1=s1: e.indirect_dma_start(
                        out=yk[s1][k], out_offset=None, in_=yg_d, in_offset=bass.IndirectOffsetOnAxis(ap=idxs[:, i, k:k + 1], axis=0)),
                        ["yg_d", ("idxs", i)], [("yk", s1, k)], q="pool")

            def e_back(i):
                s1 = i % 2
                xt_ = ("xw", s1)
                for half in range(2):
                    pk = 2 * s1 + half
                    hs = slice(half * 512, (half + 1) * 512)
                    mm(PS[pk][:, :], WdTs[:, i, :], bdn[:, hs], True, True, [("WdTs", i), "bdn"], [("ps", pk)])
                    stt("dve", xw2[s1][:, hs], xw2[s1][:, hs], ALPHA, PS[pk][:, :], ALU.mult, ALU.add, [xt_, ("ps", pk)], [xt_])
                actf(accB[s1], yk[s1][2], AF.Copy, [("yk", s1, 2), ("wsels", i)], [("accB", s1)], scale=wsels[:, i, 2:3])
                stt("dve", xw2[s1], yk[s1][0], wsels[:, i, 0:1], xw2[s1], ALU.mult, ALU.add, [("yk", s1, 0), ("wsels", i), xt_], [xt_])
                stt("dve", xw2[s1], yk[s1][1], wsels[:, i, 1:2], xw2[s1], ALU.mult, ALU.add, [("yk", s1, 1), ("wsels", i), xt_], [xt_])
                stt("dve", accB[s1], yk[s1][3], wsels[:, i, 3:4], accB[s1], ALU.mult, ALU.add, [("yk", s1, 3), ("wsels", i), ("accB", s1)], [("accB", s1)])
                tt("dve", xw2[s1], xw2[s1], accB[s1], ALU.add, [xt_, ("accB", s1)], [xt_])
                layernorm(xw2[s1], xt_, gB2, bB2, bste, mve, sm1e["rs"], sm1e["nmr"])
                o_ = dma(xdst[i * 128:(i + 1) * 128, :], xw2[s1], [xt_], ["xs_out"])
                if l == n_layers - 1:
                    fin_ops.append(o_)

            for i in range(NT + 1):
                if i < NT:
                    e_front(i)
                if i >= 1:
                    e_back(i - 1)
            P.barrier(scr)
        P.emit(final_wait_ops=fin_ops)
    return nc


def kernel(**inputs):
    x = np.asarray(inputs["x"], np.float32)
    nb = x.shape[0]
    nc = build_nc(NL, dbg=False)
    shared = {k: np.ascontiguousarray(np.asarray(v, np.float32)) for k, v in inputs.items() if k != "x"}
    in_maps = [dict(shared, x=np.ascontiguousarray(x[b])) for b in range(nb)]
    res = run_bass_kernel_spmd(nc, in_maps, core_ids=list(range(nb)))
    return np.stack([np.asarray(r["out"], np.float32) for r in res.results], axis=0)
```
